# Optimizing a Trainium2 kernel written in Bass

```python
import math
import jax, jax.numpy as jnp
from jax import lax
import numpy as np


D_MODEL = 2048
BATCH = 2
SEQ = 4096
DEPTH = 1

GRID_W = 64
CTX_LEN = 256
DA_HEADS = 8
DA_DH = 64
DA_DV = 2 * DA_DH
GLA_HEADS = 4
GLA_DK = 128
GLA_DV = 256
GLA_RANK = 16
GLA_TAU = 16.0
GLA_CHUNK = 64
PEER_HEADS = 8
PEER_NKEYS = 128
PEER_NEXP = PEER_NKEYS * PEER_NKEYS
PEER_DQ = 256
PEER_TOPK = 16
PEER_TOK_BLOCK = 128
Q_BLOCK = 128
ROPE_BASE = 10000.0
EPS = 1e-6
DA_QK_W = DA_HEADS * 2 * DA_DH
DA_V_W = DA_HEADS * DA_DV
GLA_QK_W = GLA_HEADS * GLA_DK
GLA_V_W = GLA_HEADS * GLA_DV
IN_WIDTHS = (DA_QK_W, DA_QK_W, DA_V_W, GLA_QK_W, GLA_QK_W, GLA_V_W, GLA_V_W, 2 * GLA_RANK, D_MODEL, D_MODEL)
IN_W = sum(IN_WIDTHS)

kernel_name = 'hybrid_diffattn_gla_peer_dit_block'


def rmsnorm(x, g):
    xf = x.astype(jnp.float32)
    y = xf * lax.rsqrt(jnp.mean(xf * xf, axis=-1, keepdims=True) + EPS)
    return (y * g.astype(jnp.float32)).astype(x.dtype)


def modulate(xn, shift, scale):
    return xn * (1 + scale) + shift


def split_cols(t):
    idx = [int(i) for i in np.cumsum(IN_WIDTHS)[:-1]]
    return jnp.split(t, idx, axis=-1)


def axial_rope_tables(rows):
    row = jnp.repeat(jnp.arange(rows), GRID_W).astype(jnp.float32)
    col = jnp.tile(jnp.arange(GRID_W), rows).astype(jnp.float32)
    half = DA_DH // 2
    inv = ROPE_BASE ** (-jnp.arange(0, half, 2, dtype=jnp.float32) / half)
    ar = row[:, None] * inv
    ac = col[:, None] * inv
    return (jnp.cos(ar), jnp.sin(ar), jnp.cos(ac), jnp.sin(ac))


def rotate(x, cos, sin):
    cos = cos[None, :, None, None, :]
    sin = sin[None, :, None, None, :]
    x1, x2 = jnp.split(x, 2, axis=-1)
    return jnp.concatenate([x1 * cos - x2 * sin, x2 * cos + x1 * sin], axis=-1)


def apply_axial_rope(x, rope):
    cr, sr, cc, sc = rope
    xr, xc = jnp.split(x, 2, axis=-1)
    return jnp.concatenate([rotate(xr, cr, sr), rotate(xc, cc, sc)], axis=-1).astype(x.dtype)


def diff_attention(q, k, v, lam, lam_init, subln_g):
    B, Tq = q.shape[:2]
    nb = Tq // Q_BLOCK
    qb = jnp.moveaxis(q.reshape(B, nb, Q_BLOCK, DA_HEADS, 2, DA_DH), 1, 0)
    scale = DA_DH ** -0.5

    def one_block(qblk):
        s = jnp.einsum('bqhmd,bkhmd->bhmqk', qblk, k, preferred_element_type=jnp.float32) * scale
        p = jax.nn.softmax(s, axis=-1)
        a = p[:, :, 0] - lam * p[:, :, 1]
        return jnp.einsum('bhqk,bkhv->bqhv', a.astype(v.dtype), v)

    o = jnp.moveaxis(lax.map(one_block, qb), 0, 1).reshape(B, Tq, DA_HEADS, DA_DV)
    o = rmsnorm(o, subln_g) * (1.0 - lam_init)
    return o.reshape(B, Tq, DA_V_W)


def gla_chunked(q, k, v, log_a, s0):
    B, T, H, DK = q.shape
    DV = v.shape[-1]
    n = T // GLA_CHUNK
    r = lambda t: t.reshape(B, n, GLA_CHUNK, H, t.shape[-1]).astype(jnp.float32)
    qf, kf, vf, la = r(q), r(k), r(v), r(log_a)
    b = jnp.cumsum(la, axis=2)
    b_last = b[:, :, -1:]
    q_in = qf * jnp.exp(b)
    k_in = kf * jnp.exp(-b)
    k_st = kf * jnp.exp(b_last - b)
    mask = jnp.tril(jnp.ones((GLA_CHUNK, GLA_CHUNK), dtype=bool))
    att = jnp.where(mask, jnp.einsum('bnihd,bnjhd->bnhij', q_in, k_in), 0.0)
    o_intra = jnp.einsum('bnhij,bnjhv->bnihv', att, vf)
    st_chunk = jnp.einsum('bnjhd,bnjhv->bnhdv', k_st, vf)
    dec = jnp.exp(b_last[:, :, 0])

    def step(S, xs):
        qc, dc, sc = xs
        o = jnp.einsum('bihd,bhdv->bihv', qc, S)
        S = dc[..., None] * S + sc
        return S, o

    xs = (jnp.moveaxis(q_in, 1, 0), jnp.moveaxis(dec, 1, 0), jnp.moveaxis(st_chunk, 1, 0))
    s_fin, o_inter = lax.scan(step, s0.astype(jnp.float32), xs)
    o = o_intra + jnp.moveaxis(o_inter, 0, 1)
    return o.reshape(B, T, H, DV).astype(v.dtype), s_fin


def gla_inputs(gq, gk, gv, glr, w2_f, b_f, w2_b, b_b):
    B, T = gq.shape[:2]
    hs = lambda t, d: t.reshape(B, T, GLA_HEADS, d)
    q = hs(gq, GLA_DK) * GLA_DK ** -0.5
    k = hs(gk, GLA_DK)
    v = hs(gv, GLA_DV)
    la_f = jax.nn.log_sigmoid((glr[..., :GLA_RANK] @ w2_f + b_f).astype(jnp.float32)) / GLA_TAU
    la_b = jax.nn.log_sigmoid((glr[..., GLA_RANK:] @ w2_b + b_b).astype(jnp.float32)) / GLA_TAU
    return q, k, v, hs(la_f, GLA_DK), hs(la_b, GLA_DK)


def gla_output(o, gr, norm_g):
    B, T = o.shape[:2]
    return rmsnorm(o, norm_g).reshape(B, T, GLA_V_W) * jax.nn.silu(gr)


def merge_branches(y_a, y_b, ga, gb, w_br_a, w_br_b, w_out):
    return (jax.nn.sigmoid(ga) * (y_a @ w_br_a) + jax.nn.sigmoid(gb) * (y_b @ w_br_b)) @ w_out


def flip(t):
    return jnp.flip(t, axis=1)


def token_mixer(h, hc, rope, w_in, qn_g, kn_g, lam, lam_init, subln_g,
                w2_f, b_f, w2_b, b_b, gla_norm_g, w_br_a, w_br_b, w_out, with_ctx_out):
    B, T, _ = h.shape
    Tc = hc.shape[1]
    (dq, dk, dv, gq, gk, gv, gr, glr, ga, gb) = split_cols(h @ w_in)
    (cdq, cdk, cdv, cgq, cgk, cgv, cgr, cglr, cga, cgb) = split_cols(hc @ w_in)
    da_shape = lambda t, n: t.reshape(B, n, DA_HEADS, 2, DA_DH)

    q = apply_axial_rope(rmsnorm(da_shape(dq, T), qn_g), rope)
    k_c = rmsnorm(da_shape(cdk, Tc), kn_g)
    v_c = cdv.reshape(B, Tc, DA_HEADS, DA_DV)
    k_all = jnp.concatenate([k_c, apply_axial_rope(rmsnorm(da_shape(dk, T), kn_g), rope)], axis=1)
    v_all = jnp.concatenate([v_c, dv.reshape(B, T, DA_HEADS, DA_DV)], axis=1)
    y_a = diff_attention(q, k_all, v_all, lam, lam_init, subln_g)

    q_g, k_g, v_g, la_f, la_b = gla_inputs(gq, gk, gv, glr, w2_f, b_f, w2_b, b_b)
    cq_g, ck_g, cv_g, cla_f, cla_b = gla_inputs(cgq, cgk, cgv, cglr, w2_f, b_f, w2_b, b_b)
    zero = jnp.zeros((B, GLA_HEADS, GLA_DK, GLA_DV), jnp.float32)
    o_cf, s_f = gla_chunked(cq_g, ck_g, cv_g, cla_f, zero)
    o_cb, s_b = gla_chunked(flip(cq_g), flip(ck_g), flip(cv_g), flip(cla_b), zero)
    o_f, _ = gla_chunked(q_g, k_g, v_g, la_f, s_f)
    o_b, _ = gla_chunked(flip(q_g), flip(k_g), flip(v_g), flip(la_b), s_b)
    y_b = gla_output(o_f + flip(o_b), gr, gla_norm_g)

    y = merge_branches(y_a, y_b, ga, gb, w_br_a, w_br_b, w_out)
    if not with_ctx_out:
        return y, None
    q_c = rmsnorm(da_shape(cdq, Tc), qn_g)
    y_ca = diff_attention(q_c, k_c, v_c, lam, lam_init, subln_g)
    y_cb = gla_output(o_cf + flip(o_cb), cgr, gla_norm_g)
    y_c = merge_branches(y_ca, y_cb, cga, cgb, w_br_a, w_br_b, w_out)
    return y, y_c


def peer_ffn(h, wq, keys, u, v):
    B, T, D = h.shape
    q = (h @ wq).reshape(B, T, PEER_HEADS, 2, PEER_DQ // 2)
    s = jnp.einsum('bthpd,hpkd->bthpk', q, keys, preferred_element_type=jnp.float32)
    s1, i1 = lax.top_k(s[..., 0, :], PEER_TOPK)
    s2, i2 = lax.top_k(s[..., 1, :], PEER_TOPK)
    cand = (s1[..., :, None] + s2[..., None, :]).reshape(B, T, PEER_HEADS, PEER_TOPK * PEER_TOPK)
    cidx = (i1[..., :, None] * PEER_NKEYS + i2[..., None, :]).reshape(B, T, PEER_HEADS, PEER_TOPK * PEER_TOPK)
    top_s, pos = lax.top_k(cand, PEER_TOPK)
    idx = jnp.take_along_axis(cidx, pos, axis=-1)
    g = jax.nn.softmax(top_s, axis=-1)
    nblk = (B * T) // PEER_TOK_BLOCK
    hf = h.reshape(nblk, PEER_TOK_BLOCK, D)
    idxf = idx.reshape(nblk, PEER_TOK_BLOCK, PEER_HEADS * PEER_TOPK)
    gf = g.reshape(nblk, PEER_TOK_BLOCK, PEER_HEADS * PEER_TOPK).astype(h.dtype)

    def one_block(args):
        xb, ib, gb = args
        act = jax.nn.gelu(jnp.einsum('pd,ped->pe', xb, u[ib]), approximate=False)
        return jnp.einsum('pe,ped->pd', act * gb, v[ib])

    return lax.map(one_block, (hf, idxf, gf)).reshape(B, T, D)


def setup_inputs(seed: int = 0) -> dict:
    key = jax.random.key(seed)
    ks = list(jax.random.split(key, 32))
    nrm = lambda shape, s: jax.random.normal(ks.pop(), shape, jnp.float32) * s
    D = D_MODEL
    return {
        'x': nrm((BATCH, SEQ, D), 1.0),
        'c': nrm((BATCH, D), 1.0),
        'ctx': nrm((BATCH, CTX_LEN, D), 1.0),
        'c_ctx': nrm((D,), 1.0),
        'ada_w': nrm((DEPTH, D, 6 * D), 0.5 * D ** -0.5),
        'ada_b': nrm((DEPTH, 6 * D), 0.01),
        'norm1_g': 1.0 + nrm((DEPTH, D), 0.02),
        'norm2_g': 1.0 + nrm((DEPTH, D), 0.02),
        'w_in': nrm((DEPTH, D, IN_W), D ** -0.5),
        'da_qn_g': 1.0 + nrm((DEPTH, DA_DH), 0.02),
        'da_kn_g': 1.0 + nrm((DEPTH, DA_DH), 0.02),
        'da_lam_q1': nrm((DEPTH, DA_DH), 0.1),
        'da_lam_k1': nrm((DEPTH, DA_DH), 0.1),
        'da_lam_q2': nrm((DEPTH, DA_DH), 0.1),
        'da_lam_k2': nrm((DEPTH, DA_DH), 0.1),
        'da_subln_g': 1.0 + nrm((DEPTH, DA_DV), 0.02),
        'gla_w2_f': nrm((DEPTH, GLA_RANK, GLA_QK_W), GLA_RANK ** -0.5),
        'gla_b_f': nrm((DEPTH, GLA_QK_W), 0.1),
        'gla_w2_b': nrm((DEPTH, GLA_RANK, GLA_QK_W), GLA_RANK ** -0.5),
        'gla_b_b': nrm((DEPTH, GLA_QK_W), 0.1),
        'gla_norm_g': 1.0 + nrm((DEPTH, GLA_DV), 0.02),
        'w_br_a': nrm((DEPTH, DA_V_W, D), DA_V_W ** -0.5),
        'w_br_b': nrm((DEPTH, GLA_V_W, D), GLA_V_W ** -0.5),
        'w_out': nrm((DEPTH, D, D), D ** -0.5),
        'peer_wq': nrm((DEPTH, D, PEER_HEADS * PEER_DQ), D ** -0.5),
        'peer_keys': nrm((DEPTH, PEER_HEADS, 2, PEER_NKEYS, PEER_DQ // 2), (PEER_DQ // 2) ** -0.5),
        'peer_u': nrm((DEPTH, PEER_NEXP, D), D ** -0.5),
        'peer_v': nrm((DEPTH, PEER_NEXP, D), 1.0),
    }


def reference(x, c, ctx, c_ctx, ada_w, ada_b, norm1_g, norm2_g, w_in, da_qn_g, da_kn_g,
              da_lam_q1, da_lam_k1, da_lam_q2, da_lam_k2, da_subln_g, gla_w2_f, gla_b_f,
              gla_w2_b, gla_b_b, gla_norm_g, w_br_a, w_br_b, w_out, peer_wq, peer_keys,
              peer_u, peer_v):
    T = x.shape[1]
    rows = T // GRID_W
    rope = axial_rope_tables(rows)
    for l in range(DEPTH):
        has_next = l + 1 < DEPTH
        mod = jax.nn.silu(c) @ ada_w[l] + ada_b[l]
        mod_c = jax.nn.silu(c_ctx) @ ada_w[l] + ada_b[l]
        sh1, sc1, g1, sh2, sc2, g2 = jnp.split(mod[:, None, :], 6, axis=-1)
        csh1, csc1, cg1, csh2, csc2, cg2 = jnp.split(mod_c, 6, axis=-1)
        lam_init = 0.8 - 0.6 * math.exp(-0.3 * l)
        lam = (jnp.exp(jnp.sum((da_lam_q1[l] * da_lam_k1[l]).astype(jnp.float32)))
               - jnp.exp(jnp.sum((da_lam_q2[l] * da_lam_k2[l]).astype(jnp.float32))) + lam_init)
        h = modulate(rmsnorm(x, norm1_g[l]), sh1, sc1)
        hc = modulate(rmsnorm(ctx, norm1_g[l]), csh1, csc1)
        y, y_c = token_mixer(h, hc, rope, w_in[l], da_qn_g[l], da_kn_g[l], lam, lam_init,
                             da_subln_g[l], gla_w2_f[l], gla_b_f[l], gla_w2_b[l], gla_b_b[l],
                             gla_norm_g[l], w_br_a[l], w_br_b[l], w_out[l], has_next)
        x = x + g1 * y
        x = x + g2 * peer_ffn(modulate(rmsnorm(x, norm2_g[l]), sh2, sc2),
                              peer_wq[l], peer_keys[l], peer_u[l], peer_v[l])
        if has_next:
            ctx = ctx + cg1 * y_c
            ctx = ctx + cg2 * peer_ffn(modulate(rmsnorm(ctx, norm2_g[l]), csh2, csc2),
                                       peer_wq[l], peer_keys[l], peer_u[l], peer_v[l])
    return x
```

```python
import numpy as np
import concourse.bass as bass
import concourse.mybir as mybir

F32 = mybir.dt.float32
BF = mybir.dt.bfloat16
I32 = mybir.dt.int32
U32 = mybir.dt.uint32
AF = mybir.ActivationFunctionType
ALU = mybir.AluOpType
AX = mybir.AxisListType

ENGS = ("pe", "act", "dve", "pool", "sp")
NLANES = 40


class _Op:
    __slots__ = ("eng", "idx", "fn", "dma", "lane", "lane_cnt", "deps", "signal", "cnt", "lane_waits")

    def __init__(self, eng, idx, fn, dma):
        self.eng = eng
        self.idx = idx
        self.fn = fn
        self.dma = dma
        self.lane = None
        self.lane_cnt = 0
        self.deps = []
        self.signal = False
        self.cnt = 0


class _Res:
    __slots__ = ("w", "rc", "rd")

    def __init__(self):
        self.w = None
        self.rc = {}
        self.rd = []


class Sched:
    def __init__(self, nc):
        self.nc = nc
        self.ops = {e: [] for e in ENGS}
        self.res = {}
        self.lane_last = [None] * NLANES
        self.lane_n = [0] * NLANES
        self.lane_rr = 0
        self.lane_rr_sw = 0
        self.out_dmas = []
        self.free = [(16640, 229376)]
        self.live = {}
        self.pending = []
        self.uid = 0
        self.handles = {}

    def alloc(self, name, shape, dtype, subs=None):
        esz = {F32: 4, BF: 2, I32: 4, U32: 4}[dtype]
        n = 1
        for s in shape[1:]:
            n *= s
        nbytes = (n * esz + 63) // 64 * 64
        for i, (lo, hi) in enumerate(self.free):
            if hi - lo >= nbytes:
                off = lo
                if hi - lo == nbytes:
                    self.free.pop(i)
                else:
                    self.free[i] = (lo + nbytes, hi)
                break
        else:
            raise RuntimeError(f"sbuf arena full allocating {name} {nbytes}; free={self.free}")
        self.uid += 1
        h = self.nc.alloc_sbuf_tensor_at(f"{name}_{self.uid}", list(shape), dtype, offset=off)
        self.handles[name] = h
        keys = [name] if subs is None else [f"{name}#{s}" for s in subs]
        self.live[name] = (off, off + nbytes, keys)
        inherit = []
        keep = []
        for (lo, hi, ops) in self.pending:
            if lo < off + nbytes and off < hi:
                inherit.extend(ops)
                keep.append((lo, hi, ops))
            else:
                keep.append((lo, hi, ops))
        self.pending = keep
        for k in keys:
            r = _Res()
            r.rd = list(inherit)
            self.res[k] = r
        return h

    def release(self, name):
        lo, hi, keys = self.live.pop(name)
        ops = []
        for k in keys:
            r = self.res.pop(k)
            if r.w is not None:
                ops.append(r.w)
            ops.extend(r.rc.values())
            ops.extend(r.rd)
        self.pending = [(a, b, o) for (a, b, o) in self.pending if not (a >= lo and b <= hi)]
        self.pending.append((lo, hi, ops))
        self.free.append((lo, hi))
        self.free.sort()
        merged = []
        for a, b in self.free:
            if merged and merged[-1][1] == a:
                merged[-1] = (merged[-1][0], b)
            else:
                merged.append((a, b))
        self.free = merged

    def declare(self, *keys):
        for k in keys:
            if k not in self.res:
                self.res[k] = _Res()

    def _r(self, k):
        r = self.res.get(k)
        if r is None:
            r = _Res()
            self.res[k] = r
        return r

    def op(self, eng, fn, reads=(), writes=(), dma=False, is_out=False):
        o = _Op(eng, len(self.ops[eng]), fn, dma)
        deps = []
        for k in reads:
            r = self._r(k)
            if r.w is not None:
                deps.append(r.w)
        for k in writes:
            r = self._r(k)
            cands = ([r.w] if r.w is not None else []) + list(r.rc.values()) + list(r.rd)
            for d in cands:
                if dma or d.dma or d.eng != eng or eng != "pe":
                    deps.append(d)
        if dma:
            half = NLANES // 2
            if eng == "pool":
                lane = half + self.lane_rr_sw
                self.lane_rr_sw = (self.lane_rr_sw + 1) % half
            else:
                lane = self.lane_rr
                self.lane_rr = (self.lane_rr + 1) % half
            o.lane = lane
            if self.lane_last[lane] is not None:
                deps.append(self.lane_last[lane])
            self.lane_n[lane] += 1
            o.lane_cnt = self.lane_n[lane]
            self.lane_last[lane] = o
            if is_out:
                self.out_dmas.append(o)
        seen = set()
        for d in deps:
            if d is o or id(d) in seen:
                continue
            seen.add(id(d))
            o.deps.append(d)
        for k in reads:
            r = self._r(k)
            if dma:
                r.rd.append(o)
            else:
                r.rc[eng] = o
        for k in writes:
            r = self._r(k)
            r.w = o
            r.rc = {}
            r.rd = []
        self.ops[eng].append(o)
        return o

    def emit(self):
        nc = self.nc
        for e in ENGS:
            waited_idx = {}
            waited_lane = {}
            for o in self.ops[e]:
                best = {}
                lanes = {}
                for d in o.deps:
                    if d.dma:
                        if d.lane_cnt > lanes.get(d.lane, 0):
                            lanes[d.lane] = d.lane_cnt
                    else:
                        if d.eng not in best or d.idx > best[d.eng].idx:
                            best[d.eng] = d
                o.deps = []
                for pe_, d in best.items():
                    if d.idx > waited_idx.get(pe_, -1):
                        waited_idx[pe_] = d.idx
                        d.signal = True
                        o.deps.append(d)
                o_l = []
                for lane, cntv in lanes.items():
                    if cntv > waited_lane.get(lane, 0):
                        waited_lane[lane] = cntv
                        o_l.append((lane, cntv))
                o.lane_waits = o_l
        for e in ENGS:
            c = 0
            for o in self.ops[e]:
                if not o.dma and o.signal:
                    c += 1
                    o.cnt = c
        import contextlib
        with contextlib.ExitStack() as st:
            esem = {e: st.enter_context(nc.semaphore(f"s_{e}")) for e in ENGS}
            lsem = [st.enter_context(nc.semaphore(f"l_{i}")) for i in range(NLANES)]
            block = st.enter_context(nc.Block())
            sched = self

            def run(eng_name, eng):
                waited = {}
                for o in sched.ops[eng_name]:
                    for d in o.deps:
                        eng.wait_ge(esem[d.eng], d.cnt)
                    for lane, cntv in o.lane_waits:
                        waited[("l", lane)] = 16 * cntv
                        eng.wait_ge(lsem[lane], 16 * cntv)
                    ins = o.fn(eng)
                    if o.dma:
                        ins.then_inc(lsem[o.lane], 16)
                    elif o.signal:
                        ins.then_inc(esem[eng_name], 1)
                if eng_name == "sp":
                    for lane in range(NLANES):
                        val = 16 * sched.lane_n[lane]
                        if val == 0 or waited.get(("l", lane), 0) >= val:
                            continue
                        waited[("l", lane)] = val
                        eng.wait_ge(lsem[lane], val)

            @block.tensor
            def _(eng):
                run("pe", eng)

            @block.scalar
            def _(eng):
                run("act", eng)

            @block.vector
            def _(eng):
                run("dve", eng)

            @block.gpsimd
            def _(eng):
                run("pool", eng)

            @block.sync
            def _(eng):
                run("sp", eng)

D = 2048
KC = 16
EPS = 1e-6
NT = 34
NOWN = 8
NSP = 27
_LAST_SCHED = [None]


def build(stage=99, dbg=False):
    nc = bass.Bass("TRN2", target_bir_lowering=False)
    S = Sched(nc)

    def din(name, shape, dt=F32):
        return nc.dram_tensor(name, list(shape), dt, kind="ExternalInput")

    xp = din("xp", [NT * 128, D])
    ropeC = din("ropeC", [NT * 128, 64])
    ropeS = din("ropeS", [NT * 128, 64])
    cT_d = din("cT", [128, 16, 2])
    ada_w = din("ada_w", [D, 6 * D])
    adab_col = din("adab_col", [128, 96])
    adab_row = din("adab_row", [1, 6 * D])
    n1g_col = din("n1g_col", [128, 16])
    n2g_row = din("n2g_row", [1, D])
    w_in = din("w_in", [D, 10272])
    qng_rep = din("qng_rep", [128, 64])
    kng_rep = din("kng_rep", [128, 64])
    lamp_d = din("lamp", [128, 4, 64])
    subln_col = din("subln_col", [128, 1])
    w2aug_d = din("w2aug", [33, 1024])
    gnorm_rep = din("gnorm_rep", [128, 256])
    w_br_a = din("w_br_a", [1024, D])
    w_br_b = din("w_br_b", [1024, D])
    w_out = din("w_out", [D, D])
    peer_wq = din("peer_wq", [D, D])
    keys_l = din("keys_l", [128, 16, 128])
    peer_u = din("peer_u", [16384, D])
    peer_v = din("peer_v", [16384, D])
    ident_d = din("ident", [128, 128])
    tri_d = din("tri", [128, 6, 128])
    msk_d = din("msk", [128, NSP, 2])
    iota_d = din("iota256", [128, 256])
    out_d = nc.dram_tensor("out", [NOWN * 128, D], F32, kind="ExternalOutput")
    Kd = nc.dram_tensor("Kd", [NT, 128, 1024], BF, kind="Internal")
    Vd = nc.dram_tensor("Vd", [NT, 128, 1024], BF, kind="Internal")
    hTd = nc.dram_tensor("hTd", [NT, 128, 16 * 128], BF, kind="Internal")
    u16 = nc.dram_tensor("u16", [16384, D], BF, kind="Internal")
    v16 = nc.dram_tensor("v16", [16384, D], BF, kind="Internal")
    tabkeys = ["u16#%d" % i for i in range(16)] + ["v16#%d" % i for i in range(16)]
    dbg_t = {}

    ps_h = nc.alloc_psum_tensor("ps", [128, 8, 512], F32) if hasattr(nc, "alloc_psum_tensor") else None
    ps = ps_h
    PB = ["ps%d" % i for i in range(8)]
    S.declare(*PB)

    def psb(b, n=1):
        return ps[:, b:b + n, :].rearrange("p a b -> p (a b)").bitcast(BF)

    def psf(b, n=1):
        return ps[:, b:b + n, :].rearrange("p a b -> p (a b)")

    def dma(eng, out, in_, reads, writes, is_out=False):
        S.op(eng, lambda e: e.dma_start(out=out, in_=in_), reads, writes, dma=True, is_out=is_out)

    def mm(out, lhsT, rhs, start, stop, reads, writes):
        S.op("pe", lambda e: e.matmul(out, lhsT, rhs, start=start, stop=stop), reads, writes)

    def tp(out, in_, ident, reads, writes):
        S.op("pe", lambda e: e.transpose(out, in_, ident), reads, writes)

    def act(out, in_, func, reads, writes, scale=1.0, bias=0.0, accum_out=None):
        if accum_out is None:
            S.op("act", lambda e: e.activation(out=out, in_=in_, func=func, scale=scale, bias=bias), reads, writes)
        else:
            S.op("act", lambda e: e.activation(out=out, in_=in_, func=func, scale=scale, bias=bias,
                                               accum_out=accum_out), reads, writes)

    def tt(eng, out, in0, in1, op, reads, writes):
        S.op(eng, lambda e: e.tensor_tensor(out, in0, in1, op), reads, writes)

    def ts(eng, out, in0, s1, s2, op0, op1, reads, writes):
        if s2 is None:
            S.op(eng, lambda e: e.tensor_scalar(out, in0, s1, None, op0), reads, writes)
        else:
            S.op(eng, lambda e: e.tensor_scalar(out, in0, s1, s2, op0, op1), reads, writes)

    def stt(out, in0, scalar, in1, op0, op1, reads, writes, accum_out=None):
        if accum_out is None:
            S.op("dve", lambda e: e.scalar_tensor_tensor(out, in0, scalar, in1, op0, op1), reads, writes)
        else:
            S.op("dve", lambda e: e.scalar_tensor_tensor(out, in0, scalar, in1, op0, op1, accum_out=accum_out),
                 reads, writes)

    def cp(eng, out, in_, reads, writes):
        S.op(eng, lambda e: e.tensor_copy(out, in_), reads, writes)

    def recip(out, in_, reads, writes):
        S.op("dve", lambda e: e.reciprocal(out, in_), reads, writes)

    def red(out, in_, op, reads, writes):
        S.op("dve", lambda e: e.tensor_reduce(out, in_, AX.X, op), reads, writes)

    def guard(eng, ap, key):
        n = ap.shape[-1] if len(ap.shape) > 1 else 1
        if eng == "act":
            S.op("act", lambda e: e.activation(out=gsc[:, 0:n], in_=ap, func=AF.Copy), (key,), (key, "gsc_a"))
        else:
            S.op("dve", lambda e: e.tensor_copy(gsd[:, 0:n], ap), (key,), (key, "gsc_d"))

    def memset(eng, ap, val, writes):
        S.op(eng, lambda e: e.memset(ap, val), (), writes)

    def wload(name_key, dst, src_cols, ncols, kchunks=16, eng="pool", src=None):
        srcap = src.ap().rearrange("(k p) n -> p k n", p=128)[:, :, src_cols:src_cols + ncols]
        dma(eng, dst, srcap, (), (name_key,))

    ident_f = S.alloc("ident_f", [128, 128], F32)
    gsc = S.alloc("gsc_a", [128, 16], F32)
    gsd = S.alloc("gsc_d", [128, 16], F32)
    ident_b = S.alloc("ident_b", [128, 128], BF)
    tri = S.alloc("tri", [128, 6, 128], F32)
    ones_b = S.alloc("ones_b", [128, 128], BF)
    totcol = S.alloc("totcol", [128, 2], F32)
    msk = S.alloc("msk", [128, NSP, 2], F32)
    qng = S.alloc("qng", [128, 64], F32)
    kng = S.alloc("kng", [128, 64], F32)
    gnr = S.alloc("gnr", [128, 256], F32)
    w2aug = S.alloc("w2aug", [33, 1024], F32)
    dma("sp", ident_f[:], ident_d[:, :], (), ("ident_f",))
    dma("sp", tri[:], tri_d[:, :, :], (), ("tri",))
    dma("sp", msk[:], msk_d[:, :, :], (), ("msk",))
    dma("sp", qng[:], qng_rep[:, :], (), ("qng",))
    dma("sp", kng[:], kng_rep[:, :], (), ("kng",))
    dma("sp", gnr[:], gnorm_rep[:, :], (), ("gnr",))
    dma("sp", w2aug[:], w2aug_d[:, :], (), ("w2aug",))
    cp("dve", ident_b[:], ident_f[:], ("ident_f",), ("ident_b",))
    memset("pool", ones_b[:], 1.0, ("ones_b",))
    memset("pool", totcol[:], -1.0 / 16.0, ("totcol",))

    lamp = S.alloc("lamp", [128, 4, 64], F32)
    lw = S.alloc("lw", [128, 2, 64], F32)
    lsm = S.alloc("lsm", [128, 8], F32)
    dma("sp", lamp[:], lamp_d[:, :, :], (), ("lamp",))
    tt("dve", lw[:], lamp[:, 0:4:2, :], lamp[:, 1:4:2, :], ALU.mult, ("lamp",), ("lw",))
    red(lsm[:, 0:2], lw[:], ALU.add, ("lw",), ("lsm",))
    act(lsm[:, 2:4], lsm[:, 0:2], AF.Exp, ("lsm",), ("lsm",))
    tt("dve", lsm[:, 4:5], lsm[:, 3:4], lsm[:, 2:3], ALU.subtract, ("lsm",), ("lsm",))
    ts("dve", lsm[:, 5:6], lsm[:, 4:5], -0.2, None, ALU.add, None, ("lsm",), ("lsm",))
    neglam = lsm[:, 5:6]
    sub08 = S.alloc("sub08", [128, 2], F32)
    dma("sp", sub08[:, 0:1], subln_col[:, :], (), ("sub08",))
    ts("dve", sub08[:, 1:2], sub08[:, 0:1], 0.8, None, ALU.mult, None, ("sub08",), ("sub08",))
    shf = S.alloc("shf", [128, 4], F32)
    S.op("dve", lambda e: e.tensor_reduce(shf[:, 0:1], qng[:], AX.X, ALU.max, apply_absolute_value=True),
         ("qng",), ("shf",))
    S.op("dve", lambda e: e.tensor_reduce(shf[:, 1:2], kng[:], AX.X, ALU.max, apply_absolute_value=True),
         ("kng",), ("shf",))
    tt("dve", shf[:, 2:3], shf[:, 0:1], shf[:, 1:2], ALU.mult, ("shf",), ("shf",))
    ts("dve", shf[:, 3:4], shf[:, 2:3], -8.0, None, ALU.mult, None, ("shf",), ("shf",))
    ebias = shf[:, 3:4]

    cTt = S.alloc("cTt", [128, 16, 2], F32)
    scb = S.alloc("scb", [128, 16, 2], BF)
    screp = S.alloc("screp", [128, 16, 128], BF)
    adabc = S.alloc("adabc", [128, 96], F32)
    n1gc = S.alloc("n1gc", [128, 16], F32)
    modT = S.alloc("modT", [128, 32, 2], F32)
    scl1 = S.alloc("scl1", [128, 2, 16], F32)
    shf1 = S.alloc("shf1", [128, 2, 16], F32)
    dma("sp", cTt[:], cT_d[:, :, :], (), ("cTt",))
    dma("sp", adabc[:], adab_col[:, :], (), ("adabc",))
    dma("sp", n1gc[:], n1g_col[:, :], (), ("n1gc",))
    act(scb[:], cTt[:], AF.Silu, ("cTt",), ("scb",))
    cp("dve", screp[:], scb[:, :, 0:1].to_broadcast([128, 16, 128]), ("scb",), ("screp",))
    aw = [S.alloc("aw%d" % i, [128, 16, 512], BF) for i in range(2)]
    for c in range(8):
        a = aw[c % 2]
        ak = "aw%d" % (c % 2)
        wload(ak, a[:], c * 512, 512, src=ada_w)
        for jj in range(4):
            col = (c * 4 + jj) * 2
            for k in range(16):
                mm(ps[:, 0, col:col + 2], a[:, k, jj * 128:(jj + 1) * 128], scb[:, k, :],
                   k == 0, k == 15, (ak, "scb"), ("ps0",))
    tt("dve", modT[:], ps[:, 0, 0:64].rearrange("p (a b) -> p a b", b=2),
       adabc[:, 0:32].unsqueeze(2).to_broadcast([128, 32, 2]), ALU.add, ("ps0", "adabc"), ("modT",))
    for r in range(2):
        stt(scl1[:, r, :], modT[:, 16:32, r], 1.0, n1gc[:], ALU.add, ALU.mult, ("modT", "n1gc"), ("scl1",))
        cp("dve", shf1[:, r, :], modT[:, 0:16, r], ("modT",), ("shf1",))
    S.release("aw0")
    S.release("aw1")
    for nm in ("cTt", "scb", "adabc", "n1gc", "modT", "lamp", "lw"):
        S.release(nm)

    def compute_rows():
        rows = [S.alloc("rows#%d" % i, [128, D], F32) for i in range(4)]
        for i in range(4):
            dma("sp", rows[i][:], adab_row[0:1, (2 + i) * D:(3 + i) * D].to_broadcast([128, D]), (),
                ("rows#%d" % i,))
        aw = [S.alloc("aw%d" % i, [128, 16, 512], BF) for i in range(2)]
        for c in range(8, 24):
            a = aw[c % 2]
            ak = "aw%d" % (c % 2)
            wload(ak, a[:], c * 512, 512, src=ada_w)
            sp_i = (c - 8) // 4
            cc = (c - 8) % 4
            bank = c % 2
            for k in range(16):
                mm(ps[:, bank, :], screp[:, k, :], a[:, k, :], k == 0, k == 15, (ak, "screp"), (PB[bank],))
            rk = "rows#%d" % sp_i
            tt("dve", rows[sp_i][:, cc * 512:(cc + 1) * 512], ps[:, bank, :], rows[sp_i][:, cc * 512:(cc + 1) * 512],
               ALU.add, (PB[bank], rk), (rk,))
        n2gb = S.alloc("n2gb", [128, D], F32)
        dma("sp", n2gb[:], n2g_row[0:1, :].to_broadcast([128, D]), (), ("n2gb",))
        stt(rows[2][:], rows[2][:], 1.0, n2gb[:], ALU.add, ALU.mult, ("rows#2", "n2gb"), ("rows#2",))
        S.release("n2gb")
        S.release("aw0")
        S.release("aw1")
        S.release("screp")
        return rows

    hT_own = S.alloc("hT_own", [128, 16, NOWN * 128], BF, subs=range(NOWN))

    def qk_post(tag, pbank, g_t, gkey, T, out_bf, outkey, wk):
        ka, kb, kc, ropt, kss, kraw = wk["ka"], wk["kb"], wk["kc"], wk["ropt"], wk["kss"], wk["kraw"]
        sfx = wk["sfx"]
        KA, KB, KC, ROPT, KSS, KRAW = "ka" + sfx, "kb" + sfx, "kc" + sfx, "ropt" + sfx, "kss" + sfx, "kraw" + sfx
        act(kraw[:], psf(pbank, 2), AF.Copy, [PB[pbank], PB[pbank + 1]], (KRAW,))
        pk = [KRAW]
        src = kraw[:]
        if not wk.get("rope_prefetched", False):
            dma("sp", ropt[:, 0, :], ropeC[T * 128:(T + 1) * 128, :], (), (ROPT,))
            dma("sp", ropt[:, 1, :], ropeS[T * 128:(T + 1) * 128, :], (), (ROPT,))
        act(ka[:], src, AF.Square, pk, (KA,))
        red(kss[:, 0:16], ka[:].rearrange("p (s d) -> p s d", d=64), ALU.add, (KA,), (KSS,))
        act(kss[:, 16:32], kss[:, 0:16], AF.Sqrt, (KSS,), (KSS,), scale=1.0 / 64, bias=EPS)
        recip(kss[:, 32:48], kss[:, 16:32], (KSS,), (KSS,))
        tt("dve", ka[:].rearrange("p (s d) -> p s d", d=64), src.rearrange("p (s d) -> p s d", d=64),
           kss[:, 32:48].unsqueeze(2).to_broadcast([128, 16, 64]), ALU.mult, pk + [KSS], (KA,))
        tt("pool", kb[:].rearrange("p (s d) -> p s d", d=64), ka[:].rearrange("p (s d) -> p s d", d=64),
           g_t[:].unsqueeze(1).to_broadcast([128, 16, 64]), ALU.mult, (KA, gkey), (KB,))
        tt("dve", ka[:].rearrange("p (s d) -> p s d", d=64), kb[:].rearrange("p (s d) -> p s d", d=64),
           ropt[:, 0, :].unsqueeze(1).to_broadcast([128, 16, 64]), ALU.mult, (KB, ROPT), (KA,))
        kb5 = kb[:].rearrange("p (s a h d) -> p s a h d", s=16, a=2, h=2, d=16)
        kc5 = kc[:].rearrange("p (s a h d) -> p s a h d", s=16, a=2, h=2, d=16)
        rs4 = ropt[:, 1, :].rearrange("p (a h d) -> p a h d", a=2, h=2, d=16)
        for hh in range(2):
            tt("pool", kc5[:, :, :, hh, :], kb5[:, :, :, 1 - hh, :],
               rs4[:, :, hh, :].unsqueeze(1).to_broadcast([128, 16, 2, 16]), ALU.mult, (KB, ROPT), (KC,))
        tt("dve", out_bf, ka[:], kc[:], ALU.add, (KA, KC), (outkey,))

    if stage >= 1:
        wA = S.alloc("wA", [128, 16, 2048], BF)
        for c in range(4):
            dma("pool", wA[:, :, c * 512:(c + 1) * 512],
                w_in.ap().rearrange("(k p) n -> p k n", p=128)[:, :, 1024 + c * 512:1024 + (c + 1) * 512], (), ("wA",))
        xts = [S.alloc("xt%d" % i, [128, D], F32) for i in range(2)]
        xss = [S.alloc("xs%d" % i, [128, D], BF) for i in range(2)]
        junkb = S.alloc("junkb", [128, D], BF)
        hTts = [S.alloc("hTt%d" % i, [128, 16, 128], BF) for i in range(2)]
        def mk_wk(sfx):
            return dict(sfx=sfx, ka=S.alloc("ka" + sfx, [128, 1024], F32), kb=S.alloc("kb" + sfx, [128, 1024], F32),
                        kc=S.alloc("kc" + sfx, [128, 1024], F32), ropt=S.alloc("ropt" + sfx, [128, 2, 64], F32),
                        kss=S.alloc("kss" + sfx, [128, 48], F32), kraw=S.alloc("kraw" + sfx, [128, 1024], F32))
        wks = [mk_wk("0"), mk_wk("1")]
        krbs = [S.alloc("krb%d" % i, [128, 1024], BF) for i in range(2)]
        kTts = [S.alloc("kTt%d" % i, [128, 1024], BF) for i in range(2)]
        vbts = [S.alloc("vbt%d" % i, [128, 1024], BF) for i in range(2)]
        nsts = [S.alloc("nst%d" % i, [128, 8], F32) for i in range(2)]
        def prefetch_x(T):
            par = T % 2
            dma("sp", xts[par][:], xp[T * 128:(T + 1) * 128, :], (), ("xt%d" % par,))

        def prefetch_rope(T):
            par = T % 2
            ro = wks[par]["ropt"]
            dma("sp", ro[:, 0, :], ropeC[T * 128:(T + 1) * 128, :], (), ("ropt%d" % par,))
            dma("sp", ro[:, 1, :], ropeS[T * 128:(T + 1) * 128, :], (), ("ropt%d" % par,))

        for w_ in wks:
            w_["rope_prefetched"] = True
        hT_of = {}

        def stage_a(T):
            r = 1 if T < 2 else 0
            par = T % 2
            xt, xs, xk, xsk = xts[par], xss[par], "xt%d" % par, "xs%d" % par
            nst, NST = nsts[par], "nst%d" % par
            act(junkb[:], xt[:], AF.Square, (xk,), ("junkb", NST), accum_out=nst[:, 0:1])
            guard("act", nst[:, 0:1], NST)
            act(nst[:, 1:2], nst[:, 0:1], AF.Sqrt, (NST,), (NST,), scale=1.0 / D, bias=EPS)
            recip(nst[:, 2:3], nst[:, 1:2], (NST,), (NST,))
            if T + 1 < NT:
                prefetch_x(T + 1)
            if 1 <= T <= 32:
                ci = T - 1
                src_t, dst_t = (peer_u, u16) if ci < 16 else (peer_v, v16)
                r0 = (ci % 16) * 1024
                dma("pool", dst_t[r0:r0 + 1024, :], src_t[r0:r0 + 1024, :], (), (tabkeys[ci],))
            ts("dve", xs[:], xt[:], nst[:, 2:3], None, ALU.mult, None, (xk, NST), (xsk,))
            for k in range(16):
                b = k // 8
                tp(psb(b)[:, (k % 8) * 128:(k % 8 + 1) * 128], xs[:, k * 128:(k + 1) * 128], ident_b[:],
                   (xsk, "ident_b"), (PB[b],))
            own = 2 <= T < 2 + NOWN
            if own:
                slot = T - 2
                hT = hT_own[:, :, slot * 128:(slot + 1) * 128]
                hk = "hT_own#%d" % slot
            else:
                hT = hTts[par][:]
                hk = "hTt%d" % par
            for k in range(16):
                b = k // 8
                src = psb(b)[:, (k % 8) * 128:(k % 8 + 1) * 128]
                if k % 2 == 0:
                    act(hT[:, k, :], src, AF.Identity, (PB[b], "scl1", "shf1"), (hk,),
                        scale=scl1[:, r, k:k + 1], bias=shf1[:, r, k:k + 1])
                else:
                    ts("dve", hT[:, k, :], src, scl1[:, r, k:k + 1], shf1[:, r, k:k + 1], ALU.mult, ALU.add,
                       (PB[b], "scl1", "shf1"), (hk,))
            dma("sp", hTd[T].rearrange("p (k t) -> p k t", t=128), hT, (hk,), ("hTd%d" % T,))
            hT_of[T] = (hT, hk)

        pending_kt = []

        def stage_b(T):
            par = T % 2
            hT, hk = hT_of[T]
            krb, KRB, kTt, KTT, vbt, VBT = krbs[par], "krb%d" % par, kTts[par], "kTt%d" % par, vbts[par], "vbt%d" % par
            for c in range(4):
                for k in range(16):
                    mm(ps[:, 2 + c, :], hT[:, k, :], wA[:, k, c * 512:(c + 1) * 512], k == 0, k == 15,
                       (hk, "wA"), (PB[2 + c],))
            if pending_kt:
                pending_kt.pop()()
            act(vbt[:], psf(4, 2), AF.Copy, ("ps4", "ps5"), (VBT,))
            dma("sp", Vd[T], vbt[:], (VBT,), ("Vd%d" % T,))
            qk_post("k", 2, kng, "kng", T, krb[:], KRB, wks[par])
            if T + 2 < NT:
                prefetch_rope(T + 2)

            def k_tail(T=T, krb=krb, KRB=KRB, kTt=kTt, KTT=KTT):
                for h in range(8):
                    tp(psb(6 + T % 2)[:, h * 128:(h + 1) * 128], krb[:, h * 128:(h + 1) * 128], ident_b[:],
                       (KRB, "ident_b"), (PB[6 + T % 2],))
                act(kTt[:], psb(6 + T % 2), AF.Copy, (PB[6 + T % 2],), (KTT,))
                dma("sp", Kd[T], kTt[:], (KTT,), ("Kd%d" % T,))
            pending_kt.append(k_tail)

        prefetch_x(0)
        prefetch_rope(0)
        prefetch_rope(1)
        stage_a(0)
        for T in range(NT):
            if T + 1 < NT:
                stage_a(T + 1)
            stage_b(T)
        pending_kt.pop()()
        for nm in ("wA", "xt0", "xt1", "xs0", "xs1", "junkb", "krb0", "krb1", "kTt0", "kTt1", "vbt0", "vbt1", "nst0", "nst1"):
            S.release(nm)
        for w_ in wks:
            for nm in ("ka", "kb", "kc", "ropt", "kss", "kraw"):
                S.release(nm + w_["sfx"])

    if stage >= 2:
        wB = S.alloc("wB", [128, 16, 2080], BF)
        w3 = w_in.ap().rearrange("(k p) n -> p k n", p=128)
        for c in range(4):
            dma("pool", wB[:, :, c * 512:(c + 1) * 512], w3[:, :, 3072 + c * 512:3072 + (c + 1) * 512], (), ("wB",))
        dma("pool", wB[:, :, 2048:2080], w3[:, :, 6144:6176], (), ("wB",))
        Sst = [S.alloc("S_f", [128, 4, 256], F32), S.alloc("S_b", [128, 4, 256], F32)]
        Skey = ["S_f", "S_b"]
        memset("pool", Sst[0][:], 0.0, ("S_f",))
        memset("pool", Sst[1][:], 0.0, ("S_b",))
        o_f = S.alloc("o_f", [128, NOWN, 1024], BF, subs=range(NOWN))
        ybn = S.alloc("ybn", [128, NOWN, 1024], BF, subs=range(NOWN))
        hTts2 = hTts
        glra = S.alloc("glra", [33, 128], F32)
        memset("pool", glra[:], 1.0, ("glra",))
        Lt = S.alloc("Lt", [128, 1024], F32)
        Erem = S.alloc("Erem", [128, 1024], F32)
        dec = S.alloc("dec", [128, 24], F32)
        vb = S.alloc("vb", [128, 1024], BF)
        kst = S.alloc("kst", [128, 2, 512], BF)
        E12 = S.alloc("E12", [128, 2, 512], F32)
        qkin = S.alloc("qkin", [128, 2, 512], BF)
        attm = S.alloc("attm", [128, 4, 128], BF)
        Sbf = S.alloc("Sbf", [128, 4, 256], BF)
        osum = S.alloc("osum", [128, 1024], F32)
        ojk = S.alloc("ojk", [128, 1024], F32)
        oss = S.alloc("oss", [128, 12], F32)
        gcnt = [0]

        def gla_front(T, own_slot):
            par = gcnt[0] % 2
            gcnt[0] += 1
            if own_slot is None:
                hT = hTts2[par][:]
                hk = "hTt%d" % par
                dma("sp", hT, hTd[T].rearrange("p (k t) -> p k t", t=128), ("hTd%d" % T,), (hk,))
            else:
                hT = hT_own[:, :, own_slot * 128:(own_slot + 1) * 128]
                hk = "hT_own#%d" % own_slot
            for k in range(16):
                mm(ps[:, 0, :], hT[:, k, :], wB[:, k, 512:1024], k == 0, k == 15, (hk, "wB"), ("ps0",))
            for c in range(2):
                for k in range(16):
                    mm(ps[:, 1 + c, :], hT[:, k, :], wB[:, k, 1024 + c * 512:1024 + (c + 1) * 512], k == 0, k == 15,
                       (hk, "wB"), (PB[1 + c],))
            for k in range(16):
                mm(ps[0:32, 3, 0:128], wB[:, k, 2048:2080], hT[:, k, :], k == 0, k == 15, (hk, "wB"), ("ps3",))
            act(glra[0:32, :], ps[0:32, 3, 0:128], AF.Copy, ("ps3",), ("glra",))
            for d_ in range(2):
                mm(ps[:, 4 + d_, :], glra[0:33, :], w2aug[0:33, d_ * 512:(d_ + 1) * 512], True, True,
                   ("glra", "w2aug"), (PB[4 + d_],))
            act(Lt[:], psf(4, 2), AF.Exp, ("ps4", "ps5"), ("Lt",), scale=-1.0)
            act(Lt[:], Lt[:], AF.Ln, ("Lt",), ("Lt",), bias=1.0)
            mm(ps[:, 4, :], tri[:, 1, :], Lt[:, 0:512], True, True, ("tri", "Lt"), ("ps4",))
            mm(ps[:, 5, :], tri[:, 3, :], Lt[:, 512:1024], True, True, ("tri", "Lt"), ("ps5",))
            for d_ in range(2):
                for h in range(4):
                    c0 = 128 + (d_ * 4 + h) * 2
                    mm(ps[:, 3, c0:c0 + 2], Lt[:, d_ * 512 + h * 128:d_ * 512 + (h + 1) * 128], totcol[:, 0:2],
                       True, True, ("Lt", "totcol"), ("ps3",))
            act(dec[:, 0:8], ps[:, 3, 128:144].rearrange("p (a b) -> p a b", b=2)[:, :, 0], AF.Exp, ("ps3",), ("dec",))
            act(Erem[:], psf(4, 2), AF.Exp, ("ps4", "ps5"), ("Erem",))
            act(vb[:], psf(1, 2), AF.Copy, ("ps1", "ps2"), ("vb",))
            return hT, hk

        def state_update(d_, stbank, decap):
            for h in range(4):
                mm(ps[:, stbank + h // 2, (h % 2) * 256:(h % 2 + 1) * 256], kst[:, d_, h * 128:(h + 1) * 128],
                   vb[:, h * 256:(h + 1) * 256], True, True, ("kst", "vb"), (PB[stbank + h // 2],))
            for h in range(4):
                stt(Sst[d_][:, h, :], Sst[d_][:, h, :], decap[:, h:h + 1],
                    ps[:, stbank + h // 2, (h % 2) * 256:(h % 2 + 1) * 256], ALU.mult, ALU.add,
                    (Skey[d_], "dec", PB[stbank + h // 2]), (Skey[d_],))

        def state_pass(T, pi):
            gla_front(T, None)
            for d_ in range(2):
                m = msk[:, pi, d_:d_ + 1]
                stt(kst[:, d_, :], ps[:, 0, :], m, Erem[:, d_ * 512:(d_ + 1) * 512], ALU.mult, ALU.mult,
                    ("ps0", "msk", "Erem"), ("kst",))
                ts("dve", dec[:, 8 + d_ * 4:12 + d_ * 4], dec[:, d_ * 4:d_ * 4 + 4], 1.0, m, ALU.subtract, ALU.mult,
                   ("dec", "msk"), ("dec",))
                ts("dve", dec[:, 16 + d_ * 4:20 + d_ * 4], dec[:, 8 + d_ * 4:12 + d_ * 4], 1.0, None, ALU.add, None,
                   ("dec",), ("dec",))
            state_update(0, 1, dec[:, 16:20])
            state_update(1, 6, dec[:, 20:24])

        def out_pass(slot, d_):
            T = 2 + slot
            hT, hk = gla_front(T, slot)
            for h in range(4):
                for k in range(16):
                    mm(ps[:, 6, h * 128:(h + 1) * 128], wB[:, k, h * 128:(h + 1) * 128], hT[:, k, :], k == 0, k == 15,
                       (hk, "wB"), ("ps6",))
            for h in range(4):
                for k in range(16):
                    mm(ps[:, 7, h * 128:(h + 1) * 128], wB[:, k, 512 + h * 128:512 + (h + 1) * 128], hT[:, k, :],
                       k == 0, k == 15, (hk, "wB"), ("ps7",))
            for h in range(4):
                mm(ps[:, 4, h * 128:(h + 1) * 128], Lt[:, d_ * 512 + h * 128:d_ * 512 + (h + 1) * 128],
                   tri[:, 0 if d_ == 0 else 2, :], True, True, ("Lt", "tri", "Erem"), ("ps4",))
            act(E12[:, 0, :], ps[:, 4, :], AF.Exp, ("ps4",), ("E12",))
            act(E12[:, 1, :], ps[:, 4, :], AF.Exp, ("ps4",), ("E12",), scale=-1.0)
            stt(qkin[:, 0, :], ps[:, 6, :], 128.0 ** -0.5, E12[:, 0, :], ALU.mult, ALU.mult, ("ps6", "E12"), ("qkin",))
            tt("dve", qkin[:, 1, :], ps[:, 7, :], E12[:, 1, :], ALU.mult, ("ps7", "E12"), ("qkin",))
            for h in range(4):
                mm(ps[:, 5, h * 128:(h + 1) * 128], qkin[:, 1, h * 128:(h + 1) * 128], qkin[:, 0, h * 128:(h + 1) * 128],
                   True, True, ("qkin", "Erem"), ("ps5",))
            tt("dve", attm[:], ps[:, 5, :].rearrange("p (h i) -> p h i", i=128),
               tri[:, 4 if d_ == 0 else 5, :].unsqueeze(1).to_broadcast([128, 4, 128]), ALU.mult, ("ps5", "tri"),
               ("attm",))
            cp("pool", Sbf[:], Sst[d_][:], (Skey[d_],), ("Sbf",))
            for h in range(4):
                o_ap = ps[:, 6 + h // 2, (h % 2) * 256:(h % 2 + 1) * 256]
                mm(o_ap, attm[:, h, :], vb[:, h * 256:(h + 1) * 256], True, False, ("attm", "vb", "qkin"),
                   (PB[6 + h // 2],))
                mm(o_ap, qkin[:, 0, h * 128:(h + 1) * 128], Sbf[:, h, :], False, True, ("qkin", "Sbf"),
                   (PB[6 + h // 2],))
            if d_ == 0:
                act(o_f[:, slot, :], psf(6, 2), AF.Copy, ("ps6", "ps7"), ("o_f#%d" % slot,))
            else:
                tt("dve", osum[:], psf(6, 2), o_f[:, slot, :], ALU.add, ("ps6", "ps7", "o_f#%d" % slot), ("osum",))
                act(ojk[:], osum[:], AF.Square, ("osum",), ("ojk",))
                red(oss[:, 0:4], ojk[:].rearrange("p (h v) -> p h v", v=256), ALU.add, ("ojk",), ("oss",))
                act(oss[:, 4:8], oss[:, 0:4], AF.Sqrt, ("oss",), ("oss",), scale=1.0 / 256, bias=EPS)
                recip(oss[:, 8:12], oss[:, 4:8], ("oss",), ("oss",))
                tt("dve", ojk[:].rearrange("p (h v) -> p h v", v=256), osum[:].rearrange("p (h v) -> p h v", v=256),
                   oss[:, 8:12].unsqueeze(2).to_broadcast([128, 4, 256]), ALU.mult, ("osum", "oss"), ("ojk",))
                tt("pool", ybn[:, slot, :].rearrange("p (h v) -> p h v", v=256),
                   ojk[:].rearrange("p (h v) -> p h v", v=256), gnr[:].unsqueeze(1).to_broadcast([128, 4, 256]),
                   ALU.mult, ("ojk", "gnr"), ("ybn#%d" % slot,))
            tt("dve", kst[:, d_, :], ps[:, 0, :], Erem[:, d_ * 512:(d_ + 1) * 512], ALU.mult, ("ps0", "Erem"), ("kst",))
            state_update(d_, 1, dec[:, d_ * 4:d_ * 4 + 4])

        state_pass(0, 0)
        state_pass(1, 1)
        state_pass(0, 2)
        for j in range(24):
            state_pass(2 + NOWN + j, 3 + j)
        for slot in range(NOWN):
            out_pass(slot, 0)
        for slot in reversed(range(NOWN)):
            out_pass(slot, 1)
        for nm in ("wB", "S_f", "S_b", "o_f", "glra", "Lt", "Erem", "dec", "vb", "kst", "E12", "qkin", "attm", "Sbf",
                   "osum", "ojk", "oss", "hTt0", "hTt1"):
            S.release(nm)

    if stage >= 3:
        wks = [mk_wk("0"), mk_wk("1")]
        w3 = w_in.ap().rearrange("(k p) n -> p k n", p=128)
        wQ = S.alloc("wQ", [128, 16, 1024], BF)
        for c in range(2):
            dma("pool", wQ[:, :, c * 512:(c + 1) * 512], w3[:, :, c * 512:(c + 1) * 512], (), ("wQ",))
        qT = S.alloc("qT", [128, 8, NOWN * 128], BF)
        qrb = S.alloc("qrb", [128, 1024], BF)
        for t in range(NOWN):
            hk = "hT_own#%d" % t
            for c in range(2):
                for k in range(16):
                    mm(ps[:, c, :], hT_own[:, k, t * 128:(t + 1) * 128], wQ[:, k, c * 512:(c + 1) * 512], k == 0, k == 15,
                       (hk, "wQ"), (PB[c],))
            qk_post("q", 0, qng, "qng", 2 + t, qrb[:], "qrb", wks[t % 2])
            for h in range(8):
                tp(psb(2)[:, h * 128:(h + 1) * 128], qrb[:, h * 128:(h + 1) * 128], ident_b[:], ("qrb", "ident_b"),
                   ("ps2",))
            act(qT[:, :, t * 128:(t + 1) * 128], psb(2).rearrange("p (h t) -> p h t", t=128), AF.Copy, ("ps2",), ("qT",))
        for nm in ("wQ", "qrb"):
            S.release(nm)
        for w_ in wks:
            for nm in ("ka", "kb", "kc", "ropt", "kss", "kraw"):
                S.release(nm + w_["sfx"])

        yaT = S.alloc("yaT", [128, 8, NOWN * 128], BF)
        KTh = [S.alloc("KTh%d" % i, [128, NT, 128], BF) for i in range(2)]
        Vh = [S.alloc("Vh%d" % i, [128, NT, 128], BF) for i in range(2)]
        Pt = [S.alloc("Pt%d" % i, [128, 512], BF) for i in range(4)]
        accP = [S.alloc("accP%d" % i, [128, 512], F32) for i in range(2)]
        ones_f = S.alloc("ones_f", [128, 128], F32)
        memset("pool", ones_f[:], 1.0, ("ones_f",))
        dw = [S.alloc("dw%d" % i, [128, 512], F32) for i in range(4)]
        osq = S.alloc("osq", [128, 512], BF)
        Kv = Kd.ap().rearrange("t p c -> p t c")
        Vv = Vd.ap().rearrange("t p c -> p t c")
        allK = ["Kd%d" % T for T in range(NT)]
        allV = ["Vd%d" % T for T in range(NT)]
        pcnt = 0
        for h in range(8):
            kk, vk = "KTh%d" % (h % 2), "Vh%d" % (h % 2)
            Kt, Vt = KTh[h % 2], Vh[h % 2]
            for g in range(2):
                dma("sp", Kt[:, g * 17:(g + 1) * 17, :], Kv[:, g * 17:(g + 1) * 17, h * 128:(h + 1) * 128], allK, (kk,))
                dma("sp", Vt[:, g * 17:(g + 1) * 17, :], Vv[:, g * 17:(g + 1) * 17, h * 128:(h + 1) * 128], allV, (vk,))
            for half in range(2):
                qs = slice(half * 512, (half + 1) * 512)
                its = [(m, kt) for kt in range(NT) for m in range(2)]
                slots = []
                for (m, kt) in its:
                    slots.append((pcnt % 4, pcnt % 4))
                    pcnt += 1

                def emit_score(i):
                    m, kt = its[i]
                    sb, pi_ = slots[i]
                    mm(ps[:, sb, :], Kt[64 * m:64 * m + 64, kt, :], qT[64 * m:64 * m + 64, h, qs], True, True,
                       (kk, "qT"), (PB[sb],))
                    act(Pt[pi_][:], ps[:, sb, :], AF.Exp, (PB[sb], "shf"), ("Pt%d" % pi_,), scale=0.125, bias=ebias)

                def emit_av(i):
                    m, kt = its[i]
                    sb, pi_ = slots[i]
                    mm(ps[:, 4 + m, :], Vt[:, kt, :], Pt[pi_][:], kt == 0, kt == NT - 1, (vk, "Pt%d" % pi_), (PB[4 + m],))
                    ae = "dve" if m == 0 else "pool"
                    if kt == 0:
                        cp(ae, accP[m][:], Pt[pi_][:], ("Pt%d" % pi_,), ("accP%d" % m,))
                    else:
                        tt(ae, accP[m][:], accP[m][:], Pt[pi_][:], ALU.add, ("accP%d" % m, "Pt%d" % pi_), ("accP%d" % m,))

                LOOK = 2
                for i in range(len(its) + LOOK):
                    if i < len(its):
                        emit_score(i)
                    if i >= LOOK:
                        emit_av(i - LOOK)
                for m in range(2):
                    mm(ps[:, 6 + m, :], ones_f[:], accP[m][:], True, True, ("ones_f", "accP%d" % m), (PB[6 + m],))
                    recip(dw[m][:], ps[:, 6 + m, :], (PB[6 + m],), ("dw%d" % m,))
                    tt("dve", dw[2 + m][:], ps[:, 4 + m, :], dw[m][:], ALU.mult, (PB[4 + m], "dw%d" % m),
                       ("dw%d" % (2 + m),))
                stt(dw[0][:], dw[3][:], neglam, dw[2][:], ALU.mult, ALU.add, ("dw3", "dw2", "lsm"), ("dw0",))
                act(osq[:], dw[0][:], AF.Square, ("dw0",), ("osq",))
                sb = pcnt % 4
                pcnt += 1
                mm(ps[:, sb, :], ones_b[:], osq[:], True, True, ("ones_b", "osq"), (PB[sb],))
                act(dw[1][:], ps[:, sb, :], AF.Sqrt, (PB[sb],), ("dw1",), scale=1.0 / 128, bias=EPS)
                recip(dw[2][:], dw[1][:], ("dw1",), ("dw2",))
                stt(yaT[:, h, qs], dw[0][:], sub08[:, 1:2], dw[2][:], ALU.mult, ALU.mult, ("dw0", "dw2", "sub08"),
                    ("yaT",))
        for nm in ("KTh0", "KTh1", "Vh0", "Vh1", "Pt0", "Pt1", "Pt2", "Pt3", "accP0", "accP1", "ones_f", "dw0", "dw1", "dw2", "dw3", "osq", "qT"):
            S.release(nm)

        wG = S.alloc("wG", [128, 16, 1024], BF)
        for c in range(2):
            dma("pool", wG[:, :, c * 512:(c + 1) * 512], w3[:, :, 5120 + c * 512:5120 + (c + 1) * 512], (), ("wG",))
        ybT = S.alloc("ybT", [128, 8, NOWN * 128], BF)
        sg = S.alloc("sg", [128, 1024], F32)
        ybb = S.alloc("ybb", [128, 1024], BF)
        for t in range(NOWN):
            hk = "hT_own#%d" % t
            for c in range(2):
                for k in range(16):
                    mm(ps[:, c, :], hT_own[:, k, t * 128:(t + 1) * 128], wG[:, k, c * 512:(c + 1) * 512], k == 0, k == 15,
                       (hk, "wG"), (PB[c],))
            act(sg[:], psf(0, 2), AF.Silu, ("ps0", "ps1"), ("sg",))
            tt("dve", ybb[:], sg[:], ybn[:, t, :], ALU.mult, ("sg", "ybn#%d" % t), ("ybb",))
            for c in range(8):
                tp(psb(2)[:, c * 128:(c + 1) * 128], ybb[:, c * 128:(c + 1) * 128], ident_b[:], ("ybb", "ident_b"),
                   ("ps2",))
            act(ybT[:, :, t * 128:(t + 1) * 128], psb(2).rearrange("p (h t) -> p h t", t=128), AF.Copy, ("ps2",), ("ybT",))
        for nm in ("wG", "sg", "ybb", "ybn"):
            S.release(nm)

        mT = S.alloc("mT", [128, 16, NOWN * 128], BF)
        wa3 = w_br_a.ap().rearrange("(k p) n -> p k n", p=128)
        wb3 = w_br_b.ap().rearrange("(k p) n -> p k n", p=128)
        wm = [dict(a=S.alloc("wma%d" % i, [128, 8, 256], BF), b=S.alloc("wmb%d" % i, [128, 8, 256], BF),
                   ga=S.alloc("wmga%d" % i, [128, 16, 256], BF), gb=S.alloc("wmgb%d" % i, [128, 16, 256], BF))
              for i in range(2)]
        sgt = [S.alloc("sgt%d" % i, [128, 512], F32) for i in range(4)]
        allh = ["hT_own#%d" % t for t in range(NOWN)]
        def load_wm(jg):
            w = wm[jg % 2]
            sfx = "%d" % (jg % 2)
            c0 = jg * 256
            dma("pool", w["a"][:], wa3[:, :, c0:c0 + 256], (), ("wma" + sfx,))
            dma("pool", w["b"][:], wb3[:, :, c0:c0 + 256], (), ("wmb" + sfx,))
            dma("pool", w["ga"][:], w3[:, :, 6176 + c0:6176 + c0 + 256], (), ("wmga" + sfx,))
            dma("pool", w["gb"][:], w3[:, :, 8224 + c0:8224 + c0 + 256], (), ("wmgb" + sfx,))

        load_wm(0)
        for jg in range(8):
            w = wm[jg % 2]
            sfx = "%d" % (jg % 2)
            if jg + 1 < 8:
                load_wm(jg + 1)
            for jj in range(2):
                j = jg * 2 + jj
                cs = slice(jj * 128, (jj + 1) * 128)
                for half in range(2):
                    qs = slice(half * 512, (half + 1) * 512)
                    b0 = 4 * half
                    for k in range(8):
                        mm(ps[:, b0, :], w["a"][:, k, cs], yaT[:, k, qs], k == 0, k == 7, ("wma" + sfx, "yaT"), (PB[b0],))
                    for k in range(8):
                        mm(ps[:, b0 + 1, :], w["b"][:, k, cs], ybT[:, k, qs], k == 0, k == 7, ("wmb" + sfx, "ybT"),
                           (PB[b0 + 1],))
                    for k in range(16):
                        mm(ps[:, b0 + 2, :], w["ga"][:, k, cs], hT_own[:, k, qs], k == 0, k == 15,
                           ["wmga" + sfx] + allh, (PB[b0 + 2],))
                    for k in range(16):
                        mm(ps[:, b0 + 3, :], w["gb"][:, k, cs], hT_own[:, k, qs], k == 0, k == 15,
                           ["wmgb" + sfx] + allh, (PB[b0 + 3],))
                    sa, sbb = sgt[2 * half], sgt[2 * half + 1]
                    ka_, kb_ = "sgt%d" % (2 * half), "sgt%d" % (2 * half + 1)
                    act(sa[:], ps[:, b0 + 2, :], AF.Sigmoid, (PB[b0 + 2],), (ka_,))
                    act(sbb[:], ps[:, b0 + 3, :], AF.Sigmoid, (PB[b0 + 3],), (kb_,))
                    tt("dve", sa[:], ps[:, b0, :], sa[:], ALU.mult, (PB[b0], ka_), (ka_,))
                    tt("dve", sbb[:], ps[:, b0 + 1, :], sbb[:], ALU.mult, (PB[b0 + 1], kb_), (kb_,))
                    tt("pool", mT[:, j, qs], sa[:], sbb[:], ALU.add, (ka_, kb_), ("mT",))
        for i in range(2):
            for nm in ("wma", "wmb", "wmga", "wmgb"):
                S.release("%s%d" % (nm, i))
        for nm in ("sgt0", "sgt1", "sgt2", "sgt3", "yaT", "ybT", "hT_own"):
            S.release(nm)

        rows = compute_rows()
        g1row, Qrow, Prow, g2row = rows[0][:], rows[1][:], rows[2][:], rows[3][:]
        x1 = S.alloc("x1", [128, NOWN, D], F32, subs=range(NOWN))
        for t in range(NOWN):
            dma("sp", x1[:, t, :], xp[(2 + t) * 128:(3 + t) * 128, :], (), ("x1#%d" % t,))
        wo3 = w_out.ap().rearrange("(k p) n -> p k n", p=128)
        wo = [S.alloc("wo%d" % i, [128, 16, 512], BF) for i in range(2)]
        ytmp = [S.alloc("ytmp%d" % i, [128, 512], F32) for i in range(2)]
        cnt = 0
        dma("pool", wo[0][:], wo3[:, :, 0:512], (), ("wo0",))
        for c in range(4):
            wok = "wo%d" % (c % 2)
            if c + 1 < 4:
                dma("pool", wo[(c + 1) % 2][:], wo3[:, :, (c + 1) * 512:(c + 2) * 512], (), ("wo%d" % ((c + 1) % 2),))
            for t in range(NOWN):
                bank = cnt % 4
                yt = ytmp[cnt % 2]
                yk = "ytmp%d" % (cnt % 2)
                cnt += 1
                for k in range(16):
                    mm(ps[:, bank, :], mT[:, k, t * 128:(t + 1) * 128], wo[c % 2][:, k, :], k == 0, k == 15,
                       ("mT", wok), (PB[bank],))
                tt("dve", yt[:], ps[:, bank, :], g1row[:, c * 512:(c + 1) * 512], ALU.mult, (PB[bank], "rows#0"), (yk,))
                tt("pool", x1[:, t, c * 512:(c + 1) * 512], x1[:, t, c * 512:(c + 1) * 512], yt[:], ALU.add,
                   (yk, "x1#%d" % t), ("x1#%d" % t,))
        for nm in ("wo0", "wo1", "ytmp0", "ytmp1", "rows#0"):
            S.release(nm)

    if stage >= 4:
        h2T = S.alloc("hT_own", [128, 16, NOWN * 128], BF, subs=range(NOWN))
        qpT = mT
        n2s = S.alloc("n2s", [128, 4 * NOWN], F32, subs=range(NOWN))
        h2f = S.alloc("h2f", [128, D], F32)
        h2b = S.alloc("h2b", [128, D], BF)
        for t in range(NOWN):
            xk_, nk = "x1#%d" % t, "n2s#%d" % t
            act(h2b[:], x1[:, t, :], AF.Square, (xk_,), ("h2b", nk), accum_out=n2s[:, 4 * t:4 * t + 1])
            guard("act", n2s[:, 4 * t:4 * t + 1], nk)
            act(n2s[:, 4 * t + 1:4 * t + 2], n2s[:, 4 * t:4 * t + 1], AF.Sqrt, (nk,), (nk,), scale=1.0 / D, bias=EPS)
            recip(n2s[:, 4 * t + 2:4 * t + 3], n2s[:, 4 * t + 1:4 * t + 2], (nk,), (nk,))
            stt(h2f[:], x1[:, t, :], n2s[:, 4 * t + 2:4 * t + 3], Prow, ALU.mult, ALU.mult, (xk_, nk, "rows#2"), ("h2f",))
            tt("pool", h2b[:], h2f[:], Qrow, ALU.add, ("h2f", "rows#1"), ("h2b",))
            for k in range(16):
                b = k // 8
                tp(psb(b)[:, (k % 8) * 128:(k % 8 + 1) * 128], h2b[:, k * 128:(k + 1) * 128], ident_b[:],
                   ("h2b", "ident_b"), (PB[b],))
            for b in range(2):
                act(h2T[:, 8 * b:8 * b + 8, t * 128:(t + 1) * 128], psb(b).rearrange("p (k t) -> p k t", t=128), AF.Copy,
                    (PB[b],), ("hT_own#%d" % t,))
        wq3 = peer_wq.ap().rearrange("(k p) n -> p k n", p=128)
        wq = [S.alloc("wq%d" % i, [128, 16, 256], BF) for i in range(2)]
        allh = ["hT_own#%d" % t for t in range(NOWN)]
        cnt = 0
        for c in range(8):
            wqk = "wq%d" % (c % 2)
            dma("pool", wq[c % 2][:], wq3[:, :, c * 256:(c + 1) * 256], (), (wqk,))
            for jj in range(2):
                hp = c * 2 + jj
                for half in range(2):
                    bank = 2 + cnt % 4
                    cnt += 1
                    qs = slice(half * 512, (half + 1) * 512)
                    for k in range(16):
                        mm(ps[:, bank, :], wq[c % 2][:, k, jj * 128:(jj + 1) * 128], h2T[:, k, qs], k == 0, k == 15,
                           [wqk] + allh, (PB[bank],))
                    if cnt % 2 == 0:
                        act(qpT[:, hp, qs], ps[:, bank, :], AF.Copy, (PB[bank],), ("mT",))
                    else:
                        cp("dve", qpT[:, hp, qs], ps[:, bank, :], (PB[bank],), ("mT",))
        S.release("wq0")
        S.release("wq1")
        S.release("h2b")
        S.release("hT_own")
        kl = S.alloc("kl", [128, 16, 128], F32)
        keysT = S.alloc("keysT", [128, 16, 128], BF)
        dma("sp", kl[:], keys_l[:, :, :], (), ("kl",))
        for hp in range(16):
            tp(ps[:, hp // 4, (hp % 4) * 128:(hp % 4 + 1) * 128], kl[:, hp, :], ident_f[:], ("kl", "ident_f"),
               (PB[hp // 4],))
        act(keysT[:], psf(0, 4).rearrange("p (a b) -> p a b", b=128), AF.Copy, PB[0:4], ("keysT",))
        S.release("kl")

        NEG = -1.0e30
        scs = S.alloc("scs", [128, 16, 128], F32, subs=range(16))
        scr = S.alloc("scr", [128, 16, 128], F32, subs=range(16))
        tv = S.alloc("tv", [128, 16, 16], F32, subs=range(16))
        ti = S.alloc("ti", [128, 16, 16], U32, subs=range(16))
        tif = S.alloc("tif", [128, 16, 16], F32)
        cand = scs[:].rearrange("p (h a) k -> p h (a k)", a=2)
        cand2 = scr[:].rearrange("p (h a) k -> p h (a k)", a=2)
        cv = S.alloc("cv", [128, 8, 16], F32, subs=range(8))
        cpos = S.alloc("cpos", [128, 8, 16], U32, subs=range(8))
        cab = S.alloc("cab", [128, 2, 8, 16], U32)
        cabf = S.alloc("cabf", [128, 2, 8, 16], F32)
        oh = S.alloc("oh", [128, 8, 16, 16], F32)
        esel = S.alloc("esel", [128, 2, 8, 16], F32)
        iota = S.alloc("iota", [128, 256], F32)
        dma("sp", iota[:], iota_d[:, :], (), ("iota",))
        sm = S.alloc("sm", [128, 32], F32)
        gwa = S.alloc("gwa", [128, NOWN, 128], F32, subs=range(NOWN))
        eidx = S.alloc("eidx", [128, 128], F32)
        eida = S.alloc("eida", [128, NOWN, 128], I32, subs=range(NOWN))
        memset("pool", eidx[:], 0.0, ("eidx",))
        allsc = ["scs#%d" % i for i in range(16)]
        alltv = ["tv#%d" % i for i in range(16)]
        allti = ["ti#%d" % i for i in range(16)]
        allcv = ["cv#%d" % i for i in range(8)]

        def top16(src, srck, tmp, tmpk, vals, valk, idx=None, idxk=None):
            S.op("dve", lambda e: e.max(vals[:, 0:8], src), (srck,), (valk,))
            if idx is not None:
                S.op("dve", lambda e: e.max_index(idx[:, 0:8], vals[:, 0:8], src), (srck, valk), (idxk,))
            S.op("dve", lambda e: e.match_replace(tmp, vals[:, 0:8], src, NEG), (srck, valk), (tmpk,))
            S.op("dve", lambda e: e.max(vals[:, 8:16], tmp), (tmpk,), (valk,))
            if idx is not None:
                S.op("dve", lambda e: e.max_index(idx[:, 8:16], vals[:, 8:16], tmp), (tmpk, valk), (idxk,))

        for t in range(NOWN):
            gk_ = "gwa#%d" % t
            gw = gwa[:, t, :].rearrange("p (h k) -> p h k", k=16)
            for hp in range(16):
                mm(ps[:, hp // 4, (hp % 4) * 128:(hp % 4 + 1) * 128], qpT[:, hp, t * 128:(t + 1) * 128], keysT[:, hp, :],
                   True, True, ("mT", "keysT"), (PB[hp // 4],))
            act(scs[:].rearrange("p a b -> p (a b)"), psf(0, 4), AF.Copy, PB[0:4], allsc)
            for hp in range(16):
                top16(scs[:, hp, :], "scs#%d" % hp, scr[:, hp, :], "scr#%d" % hp, tv[:, hp, :], "tv#%d" % hp,
                      ti[:, hp, :], "ti#%d" % hp)
            cp("dve", tif[:], ti[:], allti, ("tif",))
            tv4 = tv[:].rearrange("p (h a) k -> p h a k", a=2)
            ti4 = tif[:].rearrange("p (h a) k -> p h a k", a=2)
            tt("dve", cand.rearrange("p h (a b) -> p h a b", b=16), tv4[:, :, 0, :].unsqueeze(3).to_broadcast([128, 8, 16, 16]),
               tv4[:, :, 1, :].unsqueeze(2).to_broadcast([128, 8, 16, 16]), ALU.add, alltv, allsc)
            ts("dve", tif[:].rearrange("p (h a) k -> p h a k", a=2)[:, :, 0, :], ti4[:, :, 0, :], 128.0, None, ALU.mult, None,
               ("tif",), ("tif",))
            for h in range(8):
                top16(cand[:, h, :], "scs#%d" % (2 * h), cand2[:, h, :], "scr#%d" % (2 * h), cv[:, h, :], "cv#%d" % h,
                      cpos[:, h, :], "cpos#%d" % h)
            allcp = ["cpos#%d" % i for i in range(8)]
            S.op("dve", lambda e: e.tensor_single_scalar(cab[:, 0], cpos[:], 4, ALU.logical_shift_right), allcp, ("cab",))
            S.op("dve", lambda e: e.tensor_single_scalar(cab[:, 1], cpos[:], 15, ALU.bitwise_and), allcp, ("cab",))
            cp("dve", cabf[:], cab[:], ("cab",), ("cabf",))
            for w_ in range(2):
                tt("dve", oh[:], iota[:, 0:16].unsqueeze(1).unsqueeze(1).to_broadcast([128, 8, 16, 16]),
                   cabf[:, w_].unsqueeze(3).to_broadcast([128, 8, 16, 16]), ALU.is_equal, ("iota", "cabf"), ("oh",))
                tt("dve", oh[:], oh[:], ti4[:, :, w_, :].unsqueeze(2).to_broadcast([128, 8, 16, 16]), ALU.mult,
                   ("oh", "tif"), ("oh",))
                red(esel[:, w_], oh[:], ALU.add, ("oh",), ("esel",))
            tt("dve", eidx[:].rearrange("p (h k) -> p h k", k=16), esel[:, 0], esel[:, 1], ALU.add, ("esel",), ("eidx",))
            ts("dve", eidx[:], eidx[:], 0.0, 16383.0, ALU.max, ALU.min, ("eidx",), ("eidx",))
            cp("dve", eida[:, t, :], eidx[:], ("eidx",), ("eida#%d" % t,))
            ts("dve", sm[:, 0:8], cv[:, :, 0], -1.0, None, ALU.mult, None, allcv, ("sm",))
            for h in range(8):
                act(gw[:, h, :], cv[:, h, :], AF.Exp, ("cv#%d" % h, "sm"), (gk_, "sm"), bias=sm[:, h:h + 1],
                    accum_out=sm[:, 8 + h:9 + h])
            guard("act", sm[:, 8:16], "sm")
            recip(sm[:, 16:24], sm[:, 8:16], ("sm",), ("sm",))
            tt("dve", gw, gw, sm[:, 16:24].unsqueeze(2).to_broadcast([128, 8, 16]), ALU.mult, (gk_, "sm"), (gk_,))
        for nm in ("scs", "scr", "tv", "ti", "tif", "cv", "cpos", "cab", "cabf", "oh", "esel", "iota", "sm", "eidx", "keysT",
                   "mT"):
            S.release(nm)

        av = S.alloc("av", [128, 128], F32)
        coef = S.alloc("coef", [128, 128], F32)
        junk2 = S.alloc("junk2", [128, D], BF)
        memset("pool", av[:], 0.0, ("av",))
        acc = h2f
        NG = 12
        ub = [S.alloc("ub%d" % i, [128, D], BF) for i in range(NG)]
        gi = 0
        for t in range(NOWN):
            xk_, nk, ek, gk_ = "x1#%d" % t, "n2s#%d" % t, "eida#%d" % t, "gwa#%d" % t
            stt(h2f[:], x1[:, t, :], n2s[:, 4 * t + 2:4 * t + 3], Prow, ALU.mult, ALU.mult, (xk_, nk, "rows#2"), ("h2f",))
            tt("pool", h2f[:], h2f[:], Qrow, ALU.add, ("h2f", "rows#1"), ("h2f",))
            for s_ in range(128):
                u_, uk = ub[gi % NG], "ub%d" % (gi % NG)
                gi += 1
                S.op("pool", lambda e, u_=u_, s_=s_, t=t: e.indirect_dma_start(
                    out=u_[:], out_offset=None, in_=u16[:, :],
                    in_offset=bass.IndirectOffsetOnAxis(ap=eida[:, t, s_:s_ + 1], axis=0)), [ek] + tabkeys[0:16], (uk,), dma=True)
                stt(junk2[:], u_[:], 1.0, h2f[:], ALU.mult, ALU.mult, (uk, "h2f"), ("junk2", "av"),
                    accum_out=av[:, s_:s_ + 1])
            guard("dve", av[:, 120:128], "av")
            act(coef[:], av[:], AF.Gelu, ("av",), ("coef",))
            tt("dve", coef[:], coef[:], gwa[:, t, :], ALU.mult, ("coef", gk_), ("coef",))
            for s_ in range(128):
                v_, vk_ = ub[gi % NG], "ub%d" % (gi % NG)
                gi += 1
                S.op("pool", lambda e, v_=v_, s_=s_, t=t: e.indirect_dma_start(
                    out=v_[:], out_offset=None, in_=v16[:, :],
                    in_offset=bass.IndirectOffsetOnAxis(ap=eida[:, t, s_:s_ + 1], axis=0)), [ek] + tabkeys[16:32], (vk_,), dma=True)
                if s_ == 0:
                    ts("dve", acc[:], v_[:], coef[:, 0:1], None, ALU.mult, None, (vk_, "coef"), ("h2f",))
                else:
                    stt(acc[:], v_[:], coef[:, s_:s_ + 1], acc[:], ALU.mult, ALU.add, (vk_, "coef", "h2f"), ("h2f",))
            tt("dve", acc[:], acc[:], g2row, ALU.mult, ("h2f", "rows#3"), ("h2f",))
            tt("pool", h2f[:], acc[:], x1[:, t, :], ALU.add, ("h2f", xk_), ("h2f",))
            dma("sp", out_d[t * 128:(t + 1) * 128, :], h2f[:], ("h2f",), ("out%d" % t,), is_out=True)

    S.emit()
    _LAST_SCHED[0] = S
    return nc


def _host_inputs(inp):
    f = lambda a: np.ascontiguousarray(np.asarray(a, dtype=np.float32))
    x = f(inp["x"]); ctx = f(inp["ctx"]); c = f(inp["c"]); c_ctx = f(inp["c_ctx"])
    t = np.arange(4096)
    row = (t // 64).astype(np.float32); col = (t % 64).astype(np.float32)
    inv = (np.float32(10000.0) ** (-np.arange(0, 32, 2, dtype=np.float32) / np.float32(32))).astype(np.float32)
    ar = (row[:, None] * inv).astype(np.float32); ac = (col[:, None] * inv).astype(np.float32)
    cr, sr, cc, sc = np.cos(ar), np.sin(ar), np.cos(ac), np.sin(ac)
    Cx = np.concatenate([cr, cr, cc, cc], axis=1).astype(np.float32)
    Sx = np.concatenate([-sr, sr, -sc, sc], axis=1).astype(np.float32)
    jj = np.arange(128)[:, None]; ii = np.arange(128)[None, :]
    A = (jj <= ii).astype(np.float32); B = (jj > ii).astype(np.float32)
    C_ = (jj >= ii).astype(np.float32); Dm = (jj < ii).astype(np.float32)
    sc16 = np.float32(-1.0 / 16.0)
    tri = np.stack([A * sc16, B * sc16, C_ * sc16, Dm * sc16, A, C_], axis=1).astype(np.float32)
    w2aug = np.zeros((33, 1024), np.float32)
    w2aug[0:16, 0:512] = f(inp["gla_w2_f"])[0]; w2aug[16:32, 512:] = f(inp["gla_w2_b"])[0]
    w2aug[32, 0:512] = f(inp["gla_b_f"])[0]; w2aug[32, 512:] = f(inp["gla_b_b"])[0]
    shared = {
        "ada_w": f(inp["ada_w"])[0], "adab_col": f(f(inp["ada_b"])[0].reshape(96, 128).T),
        "adab_row": f(f(inp["ada_b"])[0][None]), "n1g_col": f(f(inp["norm1_g"])[0].reshape(16, 128).T),
        "n2g_row": f(f(inp["norm2_g"])[0][None]), "w_in": f(inp["w_in"])[0],
        "qng_rep": f(np.broadcast_to(f(inp["da_qn_g"])[0], (128, 64))),
        "kng_rep": f(np.broadcast_to(f(inp["da_kn_g"])[0], (128, 64))),
        "lamp": f(np.broadcast_to(np.stack([f(inp["da_lam_q1"])[0], f(inp["da_lam_k1"])[0], f(inp["da_lam_q2"])[0],
                                            f(inp["da_lam_k2"])[0]]), (128, 4, 64))),
        "subln_col": f(f(inp["da_subln_g"])[0][:, None]), "w2aug": w2aug,
        "gnorm_rep": f(np.broadcast_to(f(inp["gla_norm_g"])[0], (128, 256))),
        "w_br_a": f(inp["w_br_a"])[0], "w_br_b": f(inp["w_br_b"])[0], "w_out": f(inp["w_out"])[0],
        "peer_wq": f(inp["peer_wq"])[0],
        "keys_l": f(f(inp["peer_keys"])[0].reshape(16, 128, 128).transpose(1, 0, 2)),
        "peer_u": f(inp["peer_u"])[0], "peer_v": f(inp["peer_v"])[0],
        "ident": np.eye(128, dtype=np.float32), "tri": tri,
        "iota256": f(np.broadcast_to(np.arange(256, dtype=np.float32), (128, 256))),
    }
    maps = []
    for core in range(8):
        b, s = core // 4, core % 4
        order = list(range(8 * s, 8 * s + 8)) + list(range(0, 8 * s)) + list(range(31, 8 * s + 7, -1))
        xb = x[b].reshape(32, 128, 2048)
        xp = np.concatenate([ctx[b].reshape(2, 128, 2048), xb[order]], axis=0).reshape(34 * 128, 2048)
        rc = np.concatenate([np.ones((256, 64), np.float32), Cx.reshape(32, 128, 64)[order].reshape(4096, 64)], axis=0)
        rs = np.concatenate([np.zeros((256, 64), np.float32), Sx.reshape(32, 128, 64)[order].reshape(4096, 64)], axis=0)
        msk = np.zeros((128, 27, 2), np.float32)
        msk[:, 0, 0] = 1; msk[:, 1, :] = 1; msk[:, 2, 1] = 1
        for j in range(24):
            msk[:, 3 + j, 0 if j < 8 * s else 1] = 1
        cT = np.stack([c[b], c_ctx]).reshape(2, 16, 128).transpose(2, 1, 0)
        m = dict(shared)
        m.update({"xp": f(xp), "ropeC": f(rc), "ropeS": f(rs), "msk": msk, "cT": f(cT)})
        maps.append(m)
    return maps


_NC_CACHE = {}


def kernel(**inputs):
    from concourse.bass_utils import run_bass_kernel_spmd
    maps = _host_inputs(inputs)
    if "nc" not in _NC_CACHE:
        _NC_CACHE["nc"] = build()
    nc = _NC_CACHE["nc"]
    res = run_bass_kernel_spmd(nc, maps, core_ids=list(range(8)))
    out = np.zeros((2, 4096, 2048), np.float32)
    for core in range(8):
        b, s = core // 4, core % 4
        out[b, 1024 * s:1024 * (s + 1)] = np.asarray(res.results[core]["out"]).reshape(1024, 2048)
    return out
```

```python
import numpy as np
import concourse.bass as bass
import concourse.mybir as mybir

F32 = mybir.dt.float32
BF = mybir.dt.bfloat16
I32 = mybir.dt.int32
U32 = mybir.dt.uint32
AF = mybir.ActivationFunctionType
ALU = mybir.AluOpType
AX = mybir.AxisListType

ENGS = ("pe", "act", "dve", "pool", "sp")
NLANES = 40


class _Op:
    __slots__ = ("eng", "idx", "fn", "dma", "lane", "lane_cnt", "deps", "signal", "cnt", "lane_waits")

    def __init__(self, eng, idx, fn, dma):
        self.eng = eng
        self.idx = idx
        self.fn = fn
        self.dma = dma
        self.lane = None
        self.lane_cnt = 0
        self.deps = []
        self.signal = False
        self.cnt = 0


class _Res:
    __slots__ = ("w", "rc", "rd")

    def __init__(self):
        self.w = None
        self.rc = {}
        self.rd = []


class Sched:
    def __init__(self, nc):
        self.nc = nc
        self.ops = {e: [] for e in ENGS}
        self.res = {}
        self.lane_last = [None] * NLANES
        self.lane_n = [0] * NLANES
        self.lane_rr = 0
        self.lane_rr_sw = 0
        self.out_dmas = []
        self.free = [(16640, 229376)]
        self.live = {}
        self.pending = []
        self.uid = 0
        self.handles = {}

    def alloc(self, name, shape, dtype, subs=None):
        esz = {F32: 4, BF: 2, I32: 4, U32: 4}[dtype]
        n = 1
        for s in shape[1:]:
            n *= s
        nbytes = (n * esz + 63) // 64 * 64
        for i, (lo, hi) in enumerate(self.free):
            if hi - lo >= nbytes:
                off = lo
                if hi - lo == nbytes:
                    self.free.pop(i)
                else:
                    self.free[i] = (lo + nbytes, hi)
                break
        else:
            raise RuntimeError(f"sbuf arena full allocating {name} {nbytes}; free={self.free}")
        self.uid += 1
        h = self.nc.alloc_sbuf_tensor_at(f"{name}_{self.uid}", list(shape), dtype, offset=off)
        self.handles[name] = h
        keys = [name] if subs is None else [f"{name}#{s}" for s in subs]
        self.live[name] = (off, off + nbytes, keys)
        inherit = []
        keep = []
        for (lo, hi, ops) in self.pending:
            if lo < off + nbytes and off < hi:
                inherit.extend(ops)
                keep.append((lo, hi, ops))
            else:
                keep.append((lo, hi, ops))
        self.pending = keep
        for k in keys:
            r = _Res()
            r.rd = list(inherit)
            self.res[k] = r
        return h

    def release(self, name):
        lo, hi, keys = self.live.pop(name)
        ops = []
        for k in keys:
            r = self.res.pop(k)
            if r.w is not None:
                ops.append(r.w)
            ops.extend(r.rc.values())
            ops.extend(r.rd)
        self.pending = [(a, b, o) for (a, b, o) in self.pending if not (a >= lo and b <= hi)]
        self.pending.append((lo, hi, ops))
        self.free.append((lo, hi))
        self.free.sort()
        merged = []
        for a, b in self.free:
            if merged and merged[-1][1] == a:
                merged[-1] = (merged[-1][0], b)
            else:
                merged.append((a, b))
        self.free = merged

    def declare(self, *keys):
        for k in keys:
            if k not in self.res:
                self.res[k] = _Res()

    def _r(self, k):
        r = self.res.get(k)
        if r is None:
            r = _Res()
            self.res[k] = r
        return r

    def op(self, eng, fn, reads=(), writes=(), dma=False, is_out=False):
        o = _Op(eng, len(self.ops[eng]), fn, dma)
        deps = []
        for k in reads:
            r = self._r(k)
            if r.w is not None:
                deps.append(r.w)
        for k in writes:
            r = self._r(k)
            cands = ([r.w] if r.w is not None else []) + list(r.rc.values()) + list(r.rd)
            for d in cands:
                if dma or d.dma or d.eng != eng or eng != "pe":
                    deps.append(d)
        if dma:
            half = NLANES // 2
            if eng == "pool":
                lane = half + self.lane_rr_sw
                self.lane_rr_sw = (self.lane_rr_sw + 1) % half
            else:
                lane = self.lane_rr
                self.lane_rr = (self.lane_rr + 1) % half
            o.lane = lane
            if self.lane_last[lane] is not None:
                deps.append(self.lane_last[lane])
            self.lane_n[lane] += 1
            o.lane_cnt = self.lane_n[lane]
            self.lane_last[lane] = o
            if is_out:
                self.out_dmas.append(o)
        seen = set()
        for d in deps:
            if d is o or id(d) in seen:
                continue
            seen.add(id(d))
            o.deps.append(d)
        for k in reads:
            r = self._r(k)
            if dma:
                r.rd.append(o)
            else:
                r.rc[eng] = o
        for k in writes:
            r = self._r(k)
            r.w = o
            r.rc = {}
            r.rd = []
        self.ops[eng].append(o)
        return o

    def emit(self):
        nc = self.nc
        for e in ENGS:
            waited_idx = {}
            waited_lane = {}
            for o in self.ops[e]:
                best = {}
                lanes = {}
                for d in o.deps:
                    if d.dma:
                        if d.lane_cnt > lanes.get(d.lane, 0):
                            lanes[d.lane] = d.lane_cnt
                    else:
                        if d.eng not in best or d.idx > best[d.eng].idx:
                            best[d.eng] = d
                o.deps = []
                for pe_, d in best.items():
                    if d.idx > waited_idx.get(pe_, -1):
                        waited_idx[pe_] = d.idx
                        d.signal = True
                        o.deps.append(d)
                o_l = []
                for lane, cntv in lanes.items():
                    if cntv > waited_lane.get(lane, 0):
                        waited_lane[lane] = cntv
                        o_l.append((lane, cntv))
                o.lane_waits = o_l
        for e in ENGS:
            c = 0
            for o in self.ops[e]:
                if not o.dma and o.signal:
                    c += 1
                    o.cnt = c
        import contextlib
        with contextlib.ExitStack() as st:
            esem = {e: st.enter_context(nc.semaphore(f"s_{e}")) for e in ENGS}
            lsem = [st.enter_context(nc.semaphore(f"l_{i}")) for i in range(NLANES)]
            block = st.enter_context(nc.Block())
            sched = self

            def run(eng_name, eng):
                waited = {}
                for o in sched.ops[eng_name]:
                    for d in o.deps:
                        eng.wait_ge(esem[d.eng], d.cnt)
                    for lane, cntv in o.lane_waits:
                        waited[("l", lane)] = 16 * cntv
                        eng.wait_ge(lsem[lane], 16 * cntv)
                    ins = o.fn(eng)
                    if o.dma:
                        ins.then_inc(lsem[o.lane], 16)
                    elif o.signal:
                        ins.then_inc(esem[eng_name], 1)
                if eng_name == "sp":
                    for lane in range(NLANES):
                        val = 16 * sched.lane_n[lane]
                        if val == 0 or waited.get(("l", lane), 0) >= val:
                            continue
                        waited[("l", lane)] = val
                        eng.wait_ge(lsem[lane], val)

            @block.tensor
            def _(eng):
                run("pe", eng)

            @block.scalar
            def _(eng):
                run("act", eng)

            @block.vector
            def _(eng):
                run("dve", eng)

            @block.gpsimd
            def _(eng):
                run("pool", eng)

            @block.sync
            def _(eng):
                run("sp", eng)

D = 2048
KC = 16
EPS = 1e-6
NT = 34
NOWN = 8
NSP = 27
_LAST_SCHED = [None]


def build(stage=99, dbg=False):
    nc = bass.Bass("TRN2", target_bir_lowering=False)
    S = Sched(nc)

    def din(name, shape, dt=F32):
        return nc.dram_tensor(name, list(shape), dt, kind="ExternalInput")

    xp = din("xp", [NT * 128, D])
    ropeC = din("ropeC", [NT * 128, 64])
    ropeS = din("ropeS", [NT * 128, 64])
    cT_d = din("cT", [128, 16, 2])
    ada_w = din("ada_w", [D, 6 * D])
    adab_col = din("adab_col", [128, 96])
    adab_row = din("adab_row", [1, 6 * D])
    n1g_col = din("n1g_col", [128, 16])
    n2g_row = din("n2g_row", [1, D])
    w_in = din("w_in", [D, 10272])
    qng_rep = din("qng_rep", [128, 64])
    kng_rep = din("kng_rep", [128, 64])
    lamp_d = din("lamp", [128, 4, 64])
    subln_col = din("subln_col", [128, 1])
    w2aug_d = din("w2aug", [33, 1024])
    gnorm_rep = din("gnorm_rep", [128, 256])
    w_br_a = din("w_br_a", [1024, D])
    w_br_b = din("w_br_b", [1024, D])
    w_out = din("w_out", [D, D])
    peer_wq = din("peer_wq", [D, D])
    keys_l = din("keys_l", [128, 16, 128])
    peer_u = din("peer_u", [16384, D])
    peer_v = din("peer_v", [16384, D])
    ident_d = din("ident", [128, 128])
    tri_d = din("tri", [128, 6, 128])
    msk_d = din("msk", [128, NSP, 2])
    iota_d = din("iota256", [128, 256])
    out_d = nc.dram_tensor("out", [NOWN * 128, D], F32, kind="ExternalOutput")
    Kd = nc.dram_tensor("Kd", [NT, 128, 1024], BF, kind="Internal")
    Vd = nc.dram_tensor("Vd", [NT, 128, 1024], BF, kind="Internal")
    hTd = nc.dram_tensor("hTd", [NT, 128, 16 * 128], BF, kind="Internal")
    u16 = nc.dram_tensor("u16", [16384, D], BF, kind="Internal")
    v16 = nc.dram_tensor("v16", [16384, D], BF, kind="Internal")
    tabkeys = ["u16#%d" % i for i in range(16)] + ["v16#%d" % i for i in range(16)]
    dbg_t = {}

    ps_h = nc.alloc_psum_tensor("ps", [128, 8, 512], F32) if hasattr(nc, "alloc_psum_tensor") else None
    ps = ps_h
    PB = ["ps%d" % i for i in range(8)]
    S.declare(*PB)

    def psb(b, n=1):
        return ps[:, b:b + n, :].rearrange("p a b -> p (a b)").bitcast(BF)

    def psf(b, n=1):
        return ps[:, b:b + n, :].rearrange("p a b -> p (a b)")

    def dma(eng, out, in_, reads, writes, is_out=False):
        S.op(eng, lambda e: e.dma_start(out=out, in_=in_), reads, writes, dma=True, is_out=is_out)

    def mm(out, lhsT, rhs, start, stop, reads, writes):
        S.op("pe", lambda e: e.matmul(out, lhsT, rhs, start=start, stop=stop), reads, writes)

    def tp(out, in_, ident, reads, writes):
        S.op("pe", lambda e: e.transpose(out, in_, ident), reads, writes)

    def act(out, in_, func, reads, writes, scale=1.0, bias=0.0, accum_out=None):
        if accum_out is None:
            S.op("act", lambda e: e.activation(out=out, in_=in_, func=func, scale=scale, bias=bias), reads, writes)
        else:
            S.op("act", lambda e: e.activation(out=out, in_=in_, func=func, scale=scale, bias=bias,
                                               accum_out=accum_out), reads, writes)

    def tt(eng, out, in0, in1, op, reads, writes):
        S.op(eng, lambda e: e.tensor_tensor(out, in0, in1, op), reads, writes)

    def ts(eng, out, in0, s1, s2, op0, op1, reads, writes):
        if s2 is None:
            S.op(eng, lambda e: e.tensor_scalar(out, in0, s1, None, op0), reads, writes)
        else:
            S.op(eng, lambda e: e.tensor_scalar(out, in0, s1, s2, op0, op1), reads, writes)

    def stt(out, in0, scalar, in1, op0, op1, reads, writes, accum_out=None):
        if accum_out is None:
            S.op("dve", lambda e: e.scalar_tensor_tensor(out, in0, scalar, in1, op0, op1), reads, writes)
        else:
            S.op("dve", lambda e: e.scalar_tensor_tensor(out, in0, scalar, in1, op0, op1, accum_out=accum_out),
                 reads, writes)

    def cp(eng, out, in_, reads, writes):
        S.op(eng, lambda e: e.tensor_copy(out, in_), reads, writes)

    def recip(out, in_, reads, writes):
        S.op("dve", lambda e: e.reciprocal(out, in_), reads, writes)

    def red(out, in_, op, reads, writes):
        S.op("dve", lambda e: e.tensor_reduce(out, in_, AX.X, op), reads, writes)

    def guard(eng, ap, key):
        n = ap.shape[-1] if len(ap.shape) > 1 else 1
        if eng == "act":
            S.op("act", lambda e: e.activation(out=gsc[:, 0:n], in_=ap, func=AF.Copy), (key,), (key, "gsc_a"))
        else:
            S.op("dve", lambda e: e.tensor_copy(gsd[:, 0:n], ap), (key,), (key, "gsc_d"))

    def memset(eng, ap, val, writes):
        S.op(eng, lambda e: e.memset(ap, val), (), writes)

    def wload(name_key, dst, src_cols, ncols, kchunks=16, eng="pool", src=None):
        srcap = src.ap().rearrange("(k p) n -> p k n", p=128)[:, :, src_cols:src_cols + ncols]
        dma(eng, dst, srcap, (), (name_key,))

    ident_f = S.alloc("ident_f", [128, 128], F32)
    gsc = S.alloc("gsc_a", [128, 16], F32)
    gsd = S.alloc("gsc_d", [128, 16], F32)
    ident_b = S.alloc("ident_b", [128, 128], BF)
    tri = S.alloc("tri", [128, 6, 128], F32)
    ones_b = S.alloc("ones_b", [128, 128], BF)
    totcol = S.alloc("totcol", [128, 2], F32)
    msk = S.alloc("msk", [128, NSP, 2], F32)
    qng = S.alloc("qng", [128, 64], F32)
    kng = S.alloc("kng", [128, 64], F32)
    gnr = S.alloc("gnr", [128, 256], F32)
    w2aug = S.alloc("w2aug", [33, 1024], F32)
    dma("sp", ident_f[:], ident_d[:, :], (), ("ident_f",))
    dma("sp", tri[:], tri_d[:, :, :], (), ("tri",))
    dma("sp", msk[:], msk_d[:, :, :], (), ("msk",))
    dma("sp", qng[:], qng_rep[:, :], (), ("qng",))
    dma("sp", kng[:], kng_rep[:, :], (), ("kng",))
    dma("sp", gnr[:], gnorm_rep[:, :], (), ("gnr",))
    dma("sp", w2aug[:], w2aug_d[:, :], (), ("w2aug",))
    cp("dve", ident_b[:], ident_f[:], ("ident_f",), ("ident_b",))
    memset("pool", ones_b[:], 1.0, ("ones_b",))
    memset("pool", totcol[:], -1.0 / 16.0, ("totcol",))

    lamp = S.alloc("lamp", [128, 4, 64], F32)
    lw = S.alloc("lw", [128, 2, 64], F32)
    lsm = S.alloc("lsm", [128, 8], F32)
    dma("sp", lamp[:], lamp_d[:, :, :], (), ("lamp",))
    tt("dve", lw[:], lamp[:, 0:4:2, :], lamp[:, 1:4:2, :], ALU.mult, ("lamp",), ("lw",))
    red(lsm[:, 0:2], lw[:], ALU.add, ("lw",), ("lsm",))
    act(lsm[:, 2:4], lsm[:, 0:2], AF.Exp, ("lsm",), ("lsm",))
    tt("dve", lsm[:, 4:5], lsm[:, 3:4], lsm[:, 2:3], ALU.subtract, ("lsm",), ("lsm",))
    ts("dve", lsm[:, 5:6], lsm[:, 4:5], -0.2, None, ALU.add, None, ("lsm",), ("lsm",))
    neglam = lsm[:, 5:6]
    sub08 = S.alloc("sub08", [128, 2], F32)
    dma("sp", sub08[:, 0:1], subln_col[:, :], (), ("sub08",))
    ts("dve", sub08[:, 1:2], sub08[:, 0:1], 0.8, None, ALU.mult, None, ("sub08",), ("sub08",))
    shf = S.alloc("shf", [128, 4], F32)
    S.op("dve", lambda e: e.tensor_reduce(shf[:, 0:1], qng[:], AX.X, ALU.max, apply_absolute_value=True),
         ("qng",), ("shf",))
    S.op("dve", lambda e: e.tensor_reduce(shf[:, 1:2], kng[:], AX.X, ALU.max, apply_absolute_value=True),
         ("kng",), ("shf",))
    tt("dve", shf[:, 2:3], shf[:, 0:1], shf[:, 1:2], ALU.mult, ("shf",), ("shf",))
    ts("dve", shf[:, 3:4], shf[:, 2:3], -8.0, None, ALU.mult, None, ("shf",), ("shf",))
    ebias = shf[:, 3:4]

    cTt = S.alloc("cTt", [128, 16, 2], F32)
    scb = S.alloc("scb", [128, 16, 2], BF)
    screp = S.alloc("screp", [128, 16, 128], BF)
    adabc = S.alloc("adabc", [128, 96], F32)
    n1gc = S.alloc("n1gc", [128, 16], F32)
    modT = S.alloc("modT", [128, 32, 2], F32)
    scl1 = S.alloc("scl1", [128, 2, 16], F32)
    shf1 = S.alloc("shf1", [128, 2, 16], F32)
    dma("sp", cTt[:], cT_d[:, :, :], (), ("cTt",))
    dma("sp", adabc[:], adab_col[:, :], (), ("adabc",))
    dma("sp", n1gc[:], n1g_col[:, :], (), ("n1gc",))
    act(scb[:], cTt[:], AF.Silu, ("cTt",), ("scb",))
    cp("dve", screp[:], scb[:, :, 0:1].to_broadcast([128, 16, 128]), ("scb",), ("screp",))
    aw = [S.alloc("aw%d" % i, [128, 16, 512], BF) for i in range(2)]
    for c in range(8):
        a = aw[c % 2]
        ak = "aw%d" % (c % 2)
        wload(ak, a[:], c * 512, 512, src=ada_w)
        for jj in range(4):
            col = (c * 4 + jj) * 2
            for k in range(16):
                mm(ps[:, 0, col:col + 2], a[:, k, jj * 128:(jj + 1) * 128], scb[:, k, :],
                   k == 0, k == 15, (ak, "scb"), ("ps0",))
    tt("dve", modT[:], ps[:, 0, 0:64].rearrange("p (a b) -> p a b", b=2),
       adabc[:, 0:32].unsqueeze(2).to_broadcast([128, 32, 2]), ALU.add, ("ps0", "adabc"), ("modT",))
    for r in range(2):
        stt(scl1[:, r, :], modT[:, 16:32, r], 1.0, n1gc[:], ALU.add, ALU.mult, ("modT", "n1gc"), ("scl1",))
        cp("dve", shf1[:, r, :], modT[:, 0:16, r], ("modT",), ("shf1",))
    S.release("aw0")
    S.release("aw1")
    for nm in ("cTt", "scb", "adabc", "n1gc", "modT", "lamp", "lw"):
        S.release(nm)

    def compute_rows():
        rows = [S.alloc("rows#%d" % i, [128, D], F32) for i in range(4)]
        for i in range(4):
            dma("sp", rows[i][:], adab_row[0:1, (2 + i) * D:(3 + i) * D].to_broadcast([128, D]), (),
                ("rows#%d" % i,))
        aw = [S.alloc("aw%d" % i, [128, 16, 512], BF) for i in range(2)]
        for c in range(8, 24):
            a = aw[c % 2]
            ak = "aw%d" % (c % 2)
            wload(ak, a[:], c * 512, 512, src=ada_w)
            sp_i = (c - 8) // 4
            cc = (c - 8) % 4
            bank = c % 2
            for k in range(16):
                mm(ps[:, bank, :], screp[:, k, :], a[:, k, :], k == 0, k == 15, (ak, "screp"), (PB[bank],))
            rk = "rows#%d" % sp_i
            tt("dve", rows[sp_i][:, cc * 512:(cc + 1) * 512], ps[:, bank, :], rows[sp_i][:, cc * 512:(cc + 1) * 512],
               ALU.add, (PB[bank], rk), (rk,))
        n2gb = S.alloc("n2gb", [128, D], F32)
        dma("sp", n2gb[:], n2g_row[0:1, :].to_broadcast([128, D]), (), ("n2gb",))
        stt(rows[2][:], rows[2][:], 1.0, n2gb[:], ALU.add, ALU.mult, ("rows#2", "n2gb"), ("rows#2",))
        S.release("n2gb")
        S.release("aw0")
        S.release("aw1")
        S.release("screp")
        return rows

    hT_own = S.alloc("hT_own", [128, 16, NOWN * 128], BF, subs=range(NOWN))

    def qk_post(tag, pbank, g_t, gkey, T, out_bf, outkey, wk):
        ka, kb, kc, ropt, kss, kraw = wk["ka"], wk["kb"], wk["kc"], wk["ropt"], wk["kss"], wk["kraw"]
        sfx = wk["sfx"]
        KA, KB, KC, ROPT, KSS, KRAW = "ka" + sfx, "kb" + sfx, "kc" + sfx, "ropt" + sfx, "kss" + sfx, "kraw" + sfx
        act(kraw[:], psf(pbank, 2), AF.Copy, [PB[pbank], PB[pbank + 1]], (KRAW,))
        pk = [KRAW]
        src = kraw[:]
        if not wk.get("rope_prefetched", False):
            dma("sp", ropt[:, 0, :], ropeC[T * 128:(T + 1) * 128, :], (), (ROPT,))
            dma("sp", ropt[:, 1, :], ropeS[T * 128:(T + 1) * 128, :], (), (ROPT,))
        act(ka[:], src, AF.Square, pk, (KA,))
        red(kss[:, 0:16], ka[:].rearrange("p (s d) -> p s d", d=64), ALU.add, (KA,), (KSS,))
        act(kss[:, 16:32], kss[:, 0:16], AF.Sqrt, (KSS,), (KSS,), scale=1.0 / 64, bias=EPS)
        recip(kss[:, 32:48], kss[:, 16:32], (KSS,), (KSS,))
        tt("dve", ka[:].rearrange("p (s d) -> p s d", d=64), src.rearrange("p (s d) -> p s d", d=64),
           kss[:, 32:48].unsqueeze(2).to_broadcast([128, 16, 64]), ALU.mult, pk + [KSS], (KA,))
        tt("pool", kb[:].rearrange("p (s d) -> p s d", d=64), ka[:].rearrange("p (s d) -> p s d", d=64),
           g_t[:].unsqueeze(1).to_broadcast([128, 16, 64]), ALU.mult, (KA, gkey), (KB,))
        tt("dve", ka[:].rearrange("p (s d) -> p s d", d=64), kb[:].rearrange("p (s d) -> p s d", d=64),
           ropt[:, 0, :].unsqueeze(1).to_broadcast([128, 16, 64]), ALU.mult, (KB, ROPT), (KA,))
        kb5 = kb[:].rearrange("p (s a h d) -> p s a h d", s=16, a=2, h=2, d=16)
        kc5 = kc[:].rearrange("p (s a h d) -> p s a h d", s=16, a=2, h=2, d=16)
        rs4 = ropt[:, 1, :].rearrange("p (a h d) -> p a h d", a=2, h=2, d=16)
        for hh in range(2):
            tt("pool", kc5[:, :, :, hh, :], kb5[:, :, :, 1 - hh, :],
               rs4[:, :, hh, :].unsqueeze(1).to_broadcast([128, 16, 2, 16]), ALU.mult, (KB, ROPT), (KC,))
        tt("dve", out_bf, ka[:], kc[:], ALU.add, (KA, KC), (outkey,))

    if stage >= 1:
        wA = S.alloc("wA", [128, 16, 2048], BF)
        for c in range(4):
            dma("pool", wA[:, :, c * 512:(c + 1) * 512],
                w_in.ap().rearrange("(k p) n -> p k n", p=128)[:, :, 1024 + c * 512:1024 + (c + 1) * 512], (), ("wA",))
        xts = [S.alloc("xt%d" % i, [128, D], F32) for i in range(2)]
        xss = [S.alloc("xs%d" % i, [128, D], BF) for i in range(2)]
        junkb = S.alloc("junkb", [128, D], BF)
        hTts = [S.alloc("hTt%d" % i, [128, 16, 128], BF) for i in range(2)]
        def mk_wk(sfx):
            return dict(sfx=sfx, ka=S.alloc("ka" + sfx, [128, 1024], F32), kb=S.alloc("kb" + sfx, [128, 1024], F32),
                        kc=S.alloc("kc" + sfx, [128, 1024], F32), ropt=S.alloc("ropt" + sfx, [128, 2, 64], F32),
                        kss=S.alloc("kss" + sfx, [128, 48], F32), kraw=S.alloc("kraw" + sfx, [128, 1024], F32))
        wks = [mk_wk("0"), mk_wk("1")]
        krbs = [S.alloc("krb%d" % i, [128, 1024], BF) for i in range(2)]
        kTts = [S.alloc("kTt%d" % i, [128, 1024], BF) for i in range(2)]
        vbts = [S.alloc("vbt%d" % i, [128, 1024], BF) for i in range(2)]
        nsts = [S.alloc("nst%d" % i, [128, 8], F32) for i in range(2)]
        def prefetch_x(T):
            par = T % 2
            dma("sp", xts[par][:], xp[T * 128:(T + 1) * 128, :], (), ("xt%d" % par,))

        def prefetch_rope(T):
            par = T % 2
            ro = wks[par]["ropt"]
            dma("sp", ro[:, 0, :], ropeC[T * 128:(T + 1) * 128, :], (), ("ropt%d" % par,))
            dma("sp", ro[:, 1, :], ropeS[T * 128:(T + 1) * 128, :], (), ("ropt%d" % par,))

        for w_ in wks:
            w_["rope_prefetched"] = True
        hT_of = {}

        def stage_a(T):
            r = 1 if T < 2 else 0
            par = T % 2
            xt, xs, xk, xsk = xts[par], xss[par], "xt%d" % par, "xs%d" % par
            nst, NST = nsts[par], "nst%d" % par
            act(junkb[:], xt[:], AF.Square, (xk,), ("junkb", NST), accum_out=nst[:, 0:1])
            guard("act", nst[:, 0:1], NST)
            act(nst[:, 1:2], nst[:, 0:1], AF.Sqrt, (NST,), (NST,), scale=1.0 / D, bias=EPS)
            recip(nst[:, 2:3], nst[:, 1:2], (NST,), (NST,))
            if T + 1 < NT:
                prefetch_x(T + 1)
            if 1 <= T <= 32:
                ci = T - 1
                src_t, dst_t = (peer_u, u16) if ci < 16 else (peer_v, v16)
                r0 = (ci % 16) * 1024
                dma("pool", dst_t[r0:r0 + 1024, :], src_t[r0:r0 + 1024, :], (), (tabkeys[ci],))
            ts("dve", xs[:], xt[:], nst[:, 2:3], None, ALU.mult, None, (xk, NST), (xsk,))
            for k in range(16):
                b = k // 8
                tp(psb(b)[:, (k % 8) * 128:(k % 8 + 1) * 128], xs[:, k * 128:(k + 1) * 128], ident_b[:],
                   (xsk, "ident_b"), (PB[b],))
            own = 2 <= T < 2 + NOWN
            if own:
                slot = T - 2
                hT = hT_own[:, :, slot * 128:(slot + 1) * 128]
                hk = "hT_own#%d" % slot
            else:
                hT = hTts[par][:]
                hk = "hTt%d" % par
            for k in range(16):
                b = k // 8
                src = psb(b)[:, (k % 8) * 128:(k % 8 + 1) * 128]
                if k % 2 == 0:
                    act(hT[:, k, :], src, AF.Identity, (PB[b], "scl1", "shf1"), (hk,),
                        scale=scl1[:, r, k:k + 1], bias=shf1[:, r, k:k + 1])
                else:
                    ts("dve", hT[:, k, :], src, scl1[:, r, k:k + 1], shf1[:, r, k:k + 1], ALU.mult, ALU.add,
                       (PB[b], "scl1", "shf1"), (hk,))
            dma("sp", hTd[T].rearrange("p (k t) -> p k t", t=128), hT, (hk,), ("hTd%d" % T,))
            hT_of[T] = (hT, hk)

        pending_kt = []

        def stage_b(T):
            par = T % 2
            hT, hk = hT_of[T]
            krb, KRB, kTt, KTT, vbt, VBT = krbs[par], "krb%d" % par, kTts[par], "kTt%d" % par, vbts[par], "vbt%d" % par
            for c in range(4):
                for k in range(16):
                    mm(ps[:, 2 + c, :], hT[:, k, :], wA[:, k, c * 512:(c + 1) * 512], k == 0, k == 15,
                       (hk, "wA"), (PB[2 + c],))
            if pending_kt:
                pending_kt.pop()()
            act(vbt[:], psf(4, 2), AF.Copy, ("ps4", "ps5"), (VBT,))
            dma("sp", Vd[T], vbt[:], (VBT,), ("Vd%d" % T,))
            qk_post("k", 2, kng, "kng", T, krb[:], KRB, wks[par])
            if T + 2 < NT:
                prefetch_rope(T + 2)

            def k_tail(T=T, krb=krb, KRB=KRB, kTt=kTt, KTT=KTT):
                for h in range(8):
                    tp(psb(6 + T % 2)[:, h * 128:(h + 1) * 128], krb[:, h * 128:(h + 1) * 128], ident_b[:],
                       (KRB, "ident_b"), (PB[6 + T % 2],))
                act(kTt[:], psb(6 + T % 2), AF.Copy, (PB[6 + T % 2],), (KTT,))
                dma("sp", Kd[T], kTt[:], (KTT,), ("Kd%d" % T,))
            pending_kt.append(k_tail)

        prefetch_x(0)
        prefetch_rope(0)
        prefetch_rope(1)
        stage_a(0)
        for T in range(NT):
            if T + 1 < NT:
                stage_a(T + 1)
            stage_b(T)
        pending_kt.pop()()
        for nm in ("wA", "xt0", "xt1", "xs0", "xs1", "junkb", "krb0", "krb1", "kTt0", "kTt1", "vbt0", "vbt1", "nst0", "nst1"):
            S.release(nm)
        for w_ in wks:
            for nm in ("ka", "kb", "kc", "ropt", "kss", "kraw"):
                S.release(nm + w_["sfx"])

    if stage >= 2:
        wB = S.alloc("wB", [128, 16, 2080], BF)
        w3 = w_in.ap().rearrange("(k p) n -> p k n", p=128)
        for c in range(4):
            dma("pool", wB[:, :, c * 512:(c + 1) * 512], w3[:, :, 3072 + c * 512:3072 + (c + 1) * 512], (), ("wB",))
        dma("pool", wB[:, :, 2048:2080], w3[:, :, 6144:6176], (), ("wB",))
        Sst = [S.alloc("S_f", [128, 4, 256], F32), S.alloc("S_b", [128, 4, 256], F32)]
        Skey = ["S_f", "S_b"]
        memset("pool", Sst[0][:], 0.0, ("S_f",))
        memset("pool", Sst[1][:], 0.0, ("S_b",))
        o_f = S.alloc("o_f", [128, NOWN, 1024], BF, subs=range(NOWN))
        ybn = S.alloc("ybn", [128, NOWN, 1024], BF, subs=range(NOWN))
        hTts2 = hTts
        glra = S.alloc("glra", [33, 128], F32)
        memset("pool", glra[:], 1.0, ("glra",))
        Lt = S.alloc("Lt", [128, 1024], F32)
        Erem = S.alloc("Erem", [128, 1024], F32)
        dec = S.alloc("dec", [128, 24], F32)
        vb = S.alloc("vb", [128, 1024], BF)
        kst = S.alloc("kst", [128, 2, 512], BF)
        E12 = S.alloc("E12", [128, 2, 512], F32)
        qkin = S.alloc("qkin", [128, 2, 512], BF)
        attm = S.alloc("attm", [128, 4, 128], BF)
        Sbf = S.alloc("Sbf", [128, 4, 256], BF)
        osum = S.alloc("osum", [128, 1024], F32)
        ojk = S.alloc("ojk", [128, 1024], F32)
        oss = S.alloc("oss", [128, 12], F32)
        gcnt = [0]

        def gla_front(T, own_slot):
            par = gcnt[0] % 2
            gcnt[0] += 1
            if own_slot is None:
                hT = hTts2[par][:]
                hk = "hTt%d" % par
                dma("sp", hT, hTd[T].rearrange("p (k t) -> p k t", t=128), ("hTd%d" % T,), (hk,))
            else:
                hT = hT_own[:, :, own_slot * 128:(own_slot + 1) * 128]
                hk = "hT_own#%d" % own_slot
            for k in range(16):
                mm(ps[:, 0, :], hT[:, k, :], wB[:, k, 512:1024], k == 0, k == 15, (hk, "wB"), ("ps0",))
            for c in range(2):
                for k in range(16):
                    mm(ps[:, 1 + c, :], hT[:, k, :], wB[:, k, 1024 + c * 512:1024 + (c + 1) * 512], k == 0, k == 15,
                       (hk, "wB"), (PB[1 + c],))
            for k in range(16):
                mm(ps[0:32, 3, 0:128], wB[:, k, 2048:2080], hT[:, k, :], k == 0, k == 15, (hk, "wB"), ("ps3",))
            act(glra[0:32, :], ps[0:32, 3, 0:128], AF.Copy, ("ps3",), ("glra",))
            for d_ in range(2):
                mm(ps[:, 4 + d_, :], glra[0:33, :], w2aug[0:33, d_ * 512:(d_ + 1) * 512], True, True,
                   ("glra", "w2aug"), (PB[4 + d_],))
            act(Lt[:], psf(4, 2), AF.Exp, ("ps4", "ps5"), ("Lt",), scale=-1.0)
            act(Lt[:], Lt[:], AF.Ln, ("Lt",), ("Lt",), bias=1.0)
            mm(ps[:, 4, :], tri[:, 1, :], Lt[:, 0:512], True, True, ("tri", "Lt"), ("ps4",))
            mm(ps[:, 5, :], tri[:, 3, :], Lt[:, 512:1024], True, True, ("tri", "Lt"), ("ps5",))
            for d_ in range(2):
                for h in range(4):
                    c0 = 128 + (d_ * 4 + h) * 2
                    mm(ps[:, 3, c0:c0 + 2], Lt[:, d_ * 512 + h * 128:d_ * 512 + (h + 1) * 128], totcol[:, 0:2],
                       True, True, ("Lt", "totcol"), ("ps3",))
            act(dec[:, 0:8], ps[:, 3, 128:144].rearrange("p (a b) -> p a b", b=2)[:, :, 0], AF.Exp, ("ps3",), ("dec",))
            act(Erem[:], psf(4, 2), AF.Exp, ("ps4", "ps5"), ("Erem",))
            act(vb[:], psf(1, 2), AF.Copy, ("ps1", "ps2"), ("vb",))
            return hT, hk

        def state_update(d_, stbank, decap):
            for h in range(4):
                mm(ps[:, stbank + h // 2, (h % 2) * 256:(h % 2 + 1) * 256], kst[:, d_, h * 128:(h + 1) * 128],
                   vb[:, h * 256:(h + 1) * 256], True, True, ("kst", "vb"), (PB[stbank + h // 2],))
            for h in range(4):
                stt(Sst[d_][:, h, :], Sst[d_][:, h, :], decap[:, h:h + 1],
                    ps[:, stbank + h // 2, (h % 2) * 256:(h % 2 + 1) * 256], ALU.mult, ALU.add,
                    (Skey[d_], "dec", PB[stbank + h // 2]), (Skey[d_],))

        def state_pass(T, pi):
            gla_front(T, None)
            for d_ in range(2):
                m = msk[:, pi, d_:d_ + 1]
                stt(kst[:, d_, :], ps[:, 0, :], m, Erem[:, d_ * 512:(d_ + 1) * 512], ALU.mult, ALU.mult,
                    ("ps0", "msk", "Erem"), ("kst",))
                ts("dve", dec[:, 8 + d_ * 4:12 + d_ * 4], dec[:, d_ * 4:d_ * 4 + 4], 1.0, m, ALU.subtract, ALU.mult,
                   ("dec", "msk"), ("dec",))
                ts("dve", dec[:, 16 + d_ * 4:20 + d_ * 4], dec[:, 8 + d_ * 4:12 + d_ * 4], 1.0, None, ALU.add, None,
                   ("dec",), ("dec",))
            state_update(0, 1, dec[:, 16:20])
            state_update(1, 6, dec[:, 20:24])

        def out_pass(slot, d_):
            T = 2 + slot
            hT, hk = gla_front(T, slot)
            for h in range(4):
                for k in range(16):
                    mm(ps[:, 6, h * 128:(h + 1) * 128], wB[:, k, h * 128:(h + 1) * 128], hT[:, k, :], k == 0, k == 15,
                       (hk, "wB"), ("ps6",))
            for h in range(4):
                for k in range(16):
                    mm(ps[:, 7, h * 128:(h + 1) * 128], wB[:, k, 512 + h * 128:512 + (h + 1) * 128], hT[:, k, :],
                       k == 0, k == 15, (hk, "wB"), ("ps7",))
            for h in range(4):
                mm(ps[:, 4, h * 128:(h + 1) * 128], Lt[:, d_ * 512 + h * 128:d_ * 512 + (h + 1) * 128],
                   tri[:, 0 if d_ == 0 else 2, :], True, True, ("Lt", "tri", "Erem"), ("ps4",))
            act(E12[:, 0, :], ps[:, 4, :], AF.Exp, ("ps4",), ("E12",))
            act(E12[:, 1, :], ps[:, 4, :], AF.Exp, ("ps4",), ("E12",), scale=-1.0)
            stt(qkin[:, 0, :], ps[:, 6, :], 128.0 ** -0.5, E12[:, 0, :], ALU.mult, ALU.mult, ("ps6", "E12"), ("qkin",))
            tt("dve", qkin[:, 1, :], ps[:, 7, :], E12[:, 1, :], ALU.mult, ("ps7", "E12"), ("qkin",))
            for h in range(4):
                mm(ps[:, 5, h * 128:(h + 1) * 128], qkin[:, 1, h * 128:(h + 1) * 128], qkin[:, 0, h * 128:(h + 1) * 128],
                   True, True, ("qkin", "Erem"), ("ps5",))
            tt("dve", attm[:], ps[:, 5, :].rearrange("p (h i) -> p h i", i=128),
               tri[:, 4 if d_ == 0 else 5, :].unsqueeze(1).to_broadcast([128, 4, 128]), ALU.mult, ("ps5", "tri"),
               ("attm",))
            cp("pool", Sbf[:], Sst[d_][:], (Skey[d_],), ("Sbf",))
            for h in range(4):
                o_ap = ps[:, 6 + h // 2, (h % 2) * 256:(h % 2 + 1) * 256]
                mm(o_ap, attm[:, h, :], vb[:, h * 256:(h + 1) * 256], True, False, ("attm", "vb", "qkin"),
                   (PB[6 + h // 2],))
                mm(o_ap, qkin[:, 0, h * 128:(h + 1) * 128], Sbf[:, h, :], False, True, ("qkin", "Sbf"),
                   (PB[6 + h // 2],))
            if d_ == 0:
                act(o_f[:, slot, :], psf(6, 2), AF.Copy, ("ps6", "ps7"), ("o_f#%d" % slot,))
            else:
                tt("dve", osum[:], psf(6, 2), o_f[:, slot, :], ALU.add, ("ps6", "ps7", "o_f#%d" % slot), ("osum",))
                act(ojk[:], osum[:], AF.Square, ("osum",), ("ojk",))
                red(oss[:, 0:4], ojk[:].rearrange("p (h v) -> p h v", v=256), ALU.add, ("ojk",), ("oss",))
                act(oss[:, 4:8], oss[:, 0:4], AF.Sqrt, ("oss",), ("oss",), scale=1.0 / 256, bias=EPS)
                recip(oss[:, 8:12], oss[:, 4:8], ("oss",), ("oss",))
                tt("dve", ojk[:].rearrange("p (h v) -> p h v", v=256), osum[:].rearrange("p (h v) -> p h v", v=256),
                   oss[:, 8:12].unsqueeze(2).to_broadcast([128, 4, 256]), ALU.mult, ("osum", "oss"), ("ojk",))
                tt("pool", ybn[:, slot, :].rearrange("p (h v) -> p h v", v=256),
                   ojk[:].rearrange("p (h v) -> p h v", v=256), gnr[:].unsqueeze(1).to_broadcast([128, 4, 256]),
                   ALU.mult, ("ojk", "gnr"), ("ybn#%d" % slot,))
            tt("dve", kst[:, d_, :], ps[:, 0, :], Erem[:, d_ * 512:(d_ + 1) * 512], ALU.mult, ("ps0", "Erem"), ("kst",))
            state_update(d_, 1, dec[:, d_ * 4:d_ * 4 + 4])

        state_pass(0, 0)
        state_pass(1, 1)
        state_pass(0, 2)
        for j in range(24):
            state_pass(2 + NOWN + j, 3 + j)
        for slot in range(NOWN):
            out_pass(slot, 0)
        for slot in reversed(range(NOWN)):
            out_pass(slot, 1)
        for nm in ("wB", "S_f", "S_b", "o_f", "glra", "Lt", "Erem", "dec", "vb", "kst", "E12", "qkin", "attm", "Sbf",
                   "osum", "ojk", "oss", "hTt0", "hTt1"):
            S.release(nm)

    if stage >= 3:
        wks = [mk_wk("0"), mk_wk("1")]
        w3 = w_in.ap().rearrange("(k p) n -> p k n", p=128)
        wQ = S.alloc("wQ", [128, 16, 1024], BF)
        for c in range(2):
            dma("pool", wQ[:, :, c * 512:(c + 1) * 512], w3[:, :, c * 512:(c + 1) * 512], (), ("wQ",))
        qT = S.alloc("qT", [128, 8, NOWN * 128], BF)
        qrbs = [S.alloc("qrb%d" % i, [128, 1024], BF) for i in range(2)]
        pend_q = []
        for t in range(NOWN):
            hk = "hT_own#%d" % t
            qrb, QRB = qrbs[t % 2], "qrb%d" % (t % 2)
            for c in range(2):
                for k in range(16):
                    mm(ps[:, c, :], hT_own[:, k, t * 128:(t + 1) * 128], wQ[:, k, c * 512:(c + 1) * 512], k == 0, k == 15,
                       (hk, "wQ"), (PB[c],))
            if pend_q:
                pend_q.pop()()
            qk_post("q", 0, qng, "qng", 2 + t, qrb[:], QRB, wks[t % 2])

            def q_tail(t=t, qrb=qrb, QRB=QRB):
                bk = 2 + t % 2
                for h in range(8):
                    tp(psb(bk)[:, h * 128:(h + 1) * 128], qrb[:, h * 128:(h + 1) * 128], ident_b[:], (QRB, "ident_b"),
                       (PB[bk],))
                act(qT[:, :, t * 128:(t + 1) * 128], psb(bk).rearrange("p (h t) -> p h t", t=128), AF.Copy, (PB[bk],),
                    ("qT",))
            pend_q.append(q_tail)
        pend_q.pop()()
        for nm in ("wQ", "qrb0", "qrb1"):
            S.release(nm)
        for w_ in wks:
            for nm in ("ka", "kb", "kc", "ropt", "kss", "kraw"):
                S.release(nm + w_["sfx"])

        yaT = S.alloc("yaT", [128, 8, NOWN * 128], BF)
        KTh = [S.alloc("KTh%d" % i, [128, NT, 128], BF) for i in range(2)]
        Vh = [S.alloc("Vh%d" % i, [128, NT, 128], BF) for i in range(2)]
        Pt = [S.alloc("Pt%d" % i, [128, 512], BF) for i in range(4)]
        accP = [S.alloc("accP%d" % i, [128, 512], F32) for i in range(2)]
        ones_f = S.alloc("ones_f", [128, 128], F32)
        memset("pool", ones_f[:], 1.0, ("ones_f",))
        dw = [S.alloc("dw%d" % i, [128, 512], F32) for i in range(4)]
        osq = S.alloc("osq", [128, 512], BF)
        Kv = Kd.ap().rearrange("t p c -> p t c")
        Vv = Vd.ap().rearrange("t p c -> p t c")
        allK = ["Kd%d" % T for T in range(NT)]
        allV = ["Vd%d" % T for T in range(NT)]
        pcnt = 0
        for h in range(8):
            kk, vk = "KTh%d" % (h % 2), "Vh%d" % (h % 2)
            Kt, Vt = KTh[h % 2], Vh[h % 2]
            for g in range(2):
                dma("sp", Kt[:, g * 17:(g + 1) * 17, :], Kv[:, g * 17:(g + 1) * 17, h * 128:(h + 1) * 128], allK, (kk,))
                dma("sp", Vt[:, g * 17:(g + 1) * 17, :], Vv[:, g * 17:(g + 1) * 17, h * 128:(h + 1) * 128], allV, (vk,))
            for half in range(2):
                qs = slice(half * 512, (half + 1) * 512)
                its = [(m, kt) for kt in range(NT) for m in range(2)]
                slots = []
                for (m, kt) in its:
                    slots.append((pcnt % 4, pcnt % 4))
                    pcnt += 1

                def emit_score(i):
                    m, kt = its[i]
                    sb, pi_ = slots[i]
                    mm(ps[:, sb, :], Kt[64 * m:64 * m + 64, kt, :], qT[64 * m:64 * m + 64, h, qs], True, True,
                       (kk, "qT"), (PB[sb],))
                    act(Pt[pi_][:], ps[:, sb, :], AF.Exp, (PB[sb], "shf"), ("Pt%d" % pi_,), scale=0.125, bias=ebias)

                def emit_av(i):
                    m, kt = its[i]
                    sb, pi_ = slots[i]
                    mm(ps[:, 4 + m, :], Vt[:, kt, :], Pt[pi_][:], kt == 0, kt == NT - 1, (vk, "Pt%d" % pi_), (PB[4 + m],))
                    ae = "dve" if m == 0 else "pool"
                    if kt == 0:
                        cp(ae, accP[m][:], Pt[pi_][:], ("Pt%d" % pi_,), ("accP%d" % m,))
                    else:
                        tt(ae, accP[m][:], accP[m][:], Pt[pi_][:], ALU.add, ("accP%d" % m, "Pt%d" % pi_), ("accP%d" % m,))

                LOOK = 2
                for i in range(len(its) + LOOK):
                    if i < len(its):
                        emit_score(i)
                    if i >= LOOK:
                        emit_av(i - LOOK)
                for m in range(2):
                    mm(ps[:, 6 + m, :], ones_f[:], accP[m][:], True, True, ("ones_f", "accP%d" % m), (PB[6 + m],))
                    recip(dw[m][:], ps[:, 6 + m, :], (PB[6 + m],), ("dw%d" % m,))
                    tt("dve", dw[2 + m][:], ps[:, 4 + m, :], dw[m][:], ALU.mult, (PB[4 + m], "dw%d" % m),
                       ("dw%d" % (2 + m),))
                stt(dw[0][:], dw[3][:], neglam, dw[2][:], ALU.mult, ALU.add, ("dw3", "dw2", "lsm"), ("dw0",))
                act(osq[:], dw[0][:], AF.Square, ("dw0",), ("osq",))
                sb = pcnt % 4
                pcnt += 1
                mm(ps[:, sb, :], ones_b[:], osq[:], True, True, ("ones_b", "osq"), (PB[sb],))
                act(dw[1][:], ps[:, sb, :], AF.Sqrt, (PB[sb],), ("dw1",), scale=1.0 / 128, bias=EPS)
                recip(dw[2][:], dw[1][:], ("dw1",), ("dw2",))
                stt(yaT[:, h, qs], dw[0][:], sub08[:, 1:2], dw[2][:], ALU.mult, ALU.mult, ("dw0", "dw2", "sub08"),
                    ("yaT",))
        for nm in ("KTh0", "KTh1", "Vh0", "Vh1", "Pt0", "Pt1", "Pt2", "Pt3", "accP0", "accP1", "ones_f", "dw0", "dw1", "dw2", "dw3", "osq", "qT"):
            S.release(nm)

        wG = S.alloc("wG", [128, 16, 1024], BF)
        for c in range(2):
            dma("pool", wG[:, :, c * 512:(c + 1) * 512], w3[:, :, 5120 + c * 512:5120 + (c + 1) * 512], (), ("wG",))
        ybT = S.alloc("ybT", [128, 8, NOWN * 128], BF)
        sg = S.alloc("sg", [128, 1024], F32)
        ybb = S.alloc("ybb", [128, 1024], BF)
        for t in range(NOWN):
            hk = "hT_own#%d" % t
            for c in range(2):
                for k in range(16):
                    mm(ps[:, c, :], hT_own[:, k, t * 128:(t + 1) * 128], wG[:, k, c * 512:(c + 1) * 512], k == 0, k == 15,
                       (hk, "wG"), (PB[c],))
            act(sg[:], psf(0, 2), AF.Silu, ("ps0", "ps1"), ("sg",))
            tt("dve", ybb[:], sg[:], ybn[:, t, :], ALU.mult, ("sg", "ybn#%d" % t), ("ybb",))
            for c in range(8):
                tp(psb(2)[:, c * 128:(c + 1) * 128], ybb[:, c * 128:(c + 1) * 128], ident_b[:], ("ybb", "ident_b"),
                   ("ps2",))
            act(ybT[:, :, t * 128:(t + 1) * 128], psb(2).rearrange("p (h t) -> p h t", t=128), AF.Copy, ("ps2",), ("ybT",))
        for nm in ("wG", "sg", "ybb", "ybn"):
            S.release(nm)

        mT = S.alloc("mT", [128, 16, NOWN * 128], BF)
        wa3 = w_br_a.ap().rearrange("(k p) n -> p k n", p=128)
        wb3 = w_br_b.ap().rearrange("(k p) n -> p k n", p=128)
        wm = [dict(a=S.alloc("wma%d" % i, [128, 8, 256], BF), b=S.alloc("wmb%d" % i, [128, 8, 256], BF),
                   ga=S.alloc("wmga%d" % i, [128, 16, 256], BF), gb=S.alloc("wmgb%d" % i, [128, 16, 256], BF))
              for i in range(2)]
        sgt = [S.alloc("sgt%d" % i, [128, 512], F32) for i in range(4)]
        allh = ["hT_own#%d" % t for t in range(NOWN)]
        def load_wm(jg):
            w = wm[jg % 2]
            sfx = "%d" % (jg % 2)
            c0 = jg * 256
            dma("pool", w["a"][:], wa3[:, :, c0:c0 + 256], (), ("wma" + sfx,))
            dma("pool", w["b"][:], wb3[:, :, c0:c0 + 256], (), ("wmb" + sfx,))
            dma("pool", w["ga"][:], w3[:, :, 6176 + c0:6176 + c0 + 256], (), ("wmga" + sfx,))
            dma("pool", w["gb"][:], w3[:, :, 8224 + c0:8224 + c0 + 256], (), ("wmgb" + sfx,))

        load_wm(0)
        for jg in range(8):
            w = wm[jg % 2]
            sfx = "%d" % (jg % 2)
            if jg + 1 < 8:
                load_wm(jg + 1)
            for jj in range(2):
                j = jg * 2 + jj
                cs = slice(jj * 128, (jj + 1) * 128)
                for half in range(2):
                    qs = slice(half * 512, (half + 1) * 512)
                    b0 = 4 * half
                    for k in range(8):
                        mm(ps[:, b0, :], w["a"][:, k, cs], yaT[:, k, qs], k == 0, k == 7, ("wma" + sfx, "yaT"), (PB[b0],))
                    for k in range(8):
                        mm(ps[:, b0 + 1, :], w["b"][:, k, cs], ybT[:, k, qs], k == 0, k == 7, ("wmb" + sfx, "ybT"),
                           (PB[b0 + 1],))
                    for k in range(16):
                        mm(ps[:, b0 + 2, :], w["ga"][:, k, cs], hT_own[:, k, qs], k == 0, k == 15,
                           ["wmga" + sfx] + allh, (PB[b0 + 2],))
                    for k in range(16):
                        mm(ps[:, b0 + 3, :], w["gb"][:, k, cs], hT_own[:, k, qs], k == 0, k == 15,
                           ["wmgb" + sfx] + allh, (PB[b0 + 3],))
                    sa, sbb = sgt[2 * half], sgt[2 * half + 1]
                    ka_, kb_ = "sgt%d" % (2 * half), "sgt%d" % (2 * half + 1)
                    act(sa[:], ps[:, b0 + 2, :], AF.Sigmoid, (PB[b0 + 2],), (ka_,))
                    act(sbb[:], ps[:, b0 + 3, :], AF.Sigmoid, (PB[b0 + 3],), (kb_,))
                    tt("dve", sa[:], ps[:, b0, :], sa[:], ALU.mult, (PB[b0], ka_), (ka_,))
                    tt("dve", sbb[:], ps[:, b0 + 1, :], sbb[:], ALU.mult, (PB[b0 + 1], kb_), (kb_,))
                    tt("pool", mT[:, j, qs], sa[:], sbb[:], ALU.add, (ka_, kb_), ("mT",))
        for i in range(2):
            for nm in ("wma", "wmb", "wmga", "wmgb"):
                S.release("%s%d" % (nm, i))
        for nm in ("sgt0", "sgt1", "sgt2", "sgt3", "yaT", "ybT", "hT_own"):
            S.release(nm)

        rows = compute_rows()
        g1row, Qrow, Prow, g2row = rows[0][:], rows[1][:], rows[2][:], rows[3][:]
        x1 = S.alloc("x1", [128, NOWN, D], F32, subs=range(NOWN))
        for t in range(NOWN):
            dma("sp", x1[:, t, :], xp[(2 + t) * 128:(3 + t) * 128, :], (), ("x1#%d" % t,))
        wo3 = w_out.ap().rearrange("(k p) n -> p k n", p=128)
        wo = [S.alloc("wo%d" % i, [128, 16, 512], BF) for i in range(2)]
        ytmp = [S.alloc("ytmp%d" % i, [128, 512], F32) for i in range(2)]
        cnt = 0
        dma("pool", wo[0][:], wo3[:, :, 0:512], (), ("wo0",))
        for c in range(4):
            wok = "wo%d" % (c % 2)
            if c + 1 < 4:
                dma("pool", wo[(c + 1) % 2][:], wo3[:, :, (c + 1) * 512:(c + 2) * 512], (), ("wo%d" % ((c + 1) % 2),))
            for t in range(NOWN):
                bank = cnt % 4
                yt = ytmp[cnt % 2]
                yk = "ytmp%d" % (cnt % 2)
                cnt += 1
                for k in range(16):
                    mm(ps[:, bank, :], mT[:, k, t * 128:(t + 1) * 128], wo[c % 2][:, k, :], k == 0, k == 15,
                       ("mT", wok), (PB[bank],))
                tt("dve", yt[:], ps[:, bank, :], g1row[:, c * 512:(c + 1) * 512], ALU.mult, (PB[bank], "rows#0"), (yk,))
                tt("pool", x1[:, t, c * 512:(c + 1) * 512], x1[:, t, c * 512:(c + 1) * 512], yt[:], ALU.add,
                   (yk, "x1#%d" % t), ("x1#%d" % t,))
        for nm in ("wo0", "wo1", "ytmp0", "ytmp1", "rows#0"):
            S.release(nm)

    if stage >= 4:
        h2T = S.alloc("hT_own", [128, 16, NOWN * 128], BF, subs=range(NOWN))
        qpT = mT
        n2s = S.alloc("n2s", [128, 4 * NOWN], F32, subs=range(NOWN))
        h2f = S.alloc("h2f", [128, D], F32)
        h2b = S.alloc("h2b", [128, D], BF)
        for t in range(NOWN):
            xk_, nk = "x1#%d" % t, "n2s#%d" % t
            act(h2b[:], x1[:, t, :], AF.Square, (xk_,), ("h2b", nk), accum_out=n2s[:, 4 * t:4 * t + 1])
            guard("act", n2s[:, 4 * t:4 * t + 1], nk)
            act(n2s[:, 4 * t + 1:4 * t + 2], n2s[:, 4 * t:4 * t + 1], AF.Sqrt, (nk,), (nk,), scale=1.0 / D, bias=EPS)
            recip(n2s[:, 4 * t + 2:4 * t + 3], n2s[:, 4 * t + 1:4 * t + 2], (nk,), (nk,))
            stt(h2f[:], x1[:, t, :], n2s[:, 4 * t + 2:4 * t + 3], Prow, ALU.mult, ALU.mult, (xk_, nk, "rows#2"), ("h2f",))
            tt("pool", h2b[:], h2f[:], Qrow, ALU.add, ("h2f", "rows#1"), ("h2b",))
            for k in range(16):
                b = k // 8
                tp(psb(b)[:, (k % 8) * 128:(k % 8 + 1) * 128], h2b[:, k * 128:(k + 1) * 128], ident_b[:],
                   ("h2b", "ident_b"), (PB[b],))
            for b in range(2):
                act(h2T[:, 8 * b:8 * b + 8, t * 128:(t + 1) * 128], psb(b).rearrange("p (k t) -> p k t", t=128), AF.Copy,
                    (PB[b],), ("hT_own#%d" % t,))
        wq3 = peer_wq.ap().rearrange("(k p) n -> p k n", p=128)
        wq = [S.alloc("wq%d" % i, [128, 16, 256], BF) for i in range(2)]
        allh = ["hT_own#%d" % t for t in range(NOWN)]
        cnt = 0
        for c in range(8):
            wqk = "wq%d" % (c % 2)
            dma("pool", wq[c % 2][:], wq3[:, :, c * 256:(c + 1) * 256], (), (wqk,))
            for jj in range(2):
                hp = c * 2 + jj
                for half in range(2):
                    bank = 2 + cnt % 4
                    cnt += 1
                    qs = slice(half * 512, (half + 1) * 512)
                    for k in range(16):
                        mm(ps[:, bank, :], wq[c % 2][:, k, jj * 128:(jj + 1) * 128], h2T[:, k, qs], k == 0, k == 15,
                           [wqk] + allh, (PB[bank],))
                    if cnt % 2 == 0:
                        act(qpT[:, hp, qs], ps[:, bank, :], AF.Copy, (PB[bank],), ("mT",))
                    else:
                        cp("dve", qpT[:, hp, qs], ps[:, bank, :], (PB[bank],), ("mT",))
        S.release("wq0")
        S.release("wq1")
        S.release("h2b")
        S.release("hT_own")
        kl = S.alloc("kl", [128, 16, 128], F32)
        keysT = S.alloc("keysT", [128, 16, 128], BF)
        dma("sp", kl[:], keys_l[:, :, :], (), ("kl",))
        for hp in range(16):
            tp(ps[:, hp // 4, (hp % 4) * 128:(hp % 4 + 1) * 128], kl[:, hp, :], ident_f[:], ("kl", "ident_f"),
               (PB[hp // 4],))
        act(keysT[:], psf(0, 4).rearrange("p (a b) -> p a b", b=128), AF.Copy, PB[0:4], ("keysT",))
        S.release("kl")

        NEG = -1.0e30
        scs = S.alloc("scs", [128, 16, 128], F32, subs=range(16))
        scr = S.alloc("scr", [128, 16, 128], F32, subs=range(16))
        tv = S.alloc("tv", [128, 16, 16], F32, subs=range(16))
        ti = S.alloc("ti", [128, 16, 16], U32, subs=range(16))
        tif = S.alloc("tif", [128, 16, 16], F32)
        cand = scs[:].rearrange("p (h a) k -> p h (a k)", a=2)
        cand2 = scr[:].rearrange("p (h a) k -> p h (a k)", a=2)
        cv = S.alloc("cv", [128, 8, 16], F32, subs=range(8))
        cpos = S.alloc("cpos", [128, 8, 16], U32, subs=range(8))
        cab = S.alloc("cab", [128, 2, 8, 16], U32)
        cabf = S.alloc("cabf", [128, 2, 8, 16], F32)
        oh = S.alloc("oh", [128, 8, 16, 16], F32)
        esel = S.alloc("esel", [128, 2, 8, 16], F32)
        iota = S.alloc("iota", [128, 256], F32)
        dma("sp", iota[:], iota_d[:, :], (), ("iota",))
        sm = S.alloc("sm", [128, 32], F32)
        gwa = S.alloc("gwa", [128, NOWN, 128], F32, subs=range(NOWN))
        eidx = S.alloc("eidx", [128, 128], F32)
        eida = S.alloc("eida", [128, NOWN, 128], I32, subs=range(NOWN))
        memset("pool", eidx[:], 0.0, ("eidx",))
        allsc = ["scs#%d" % i for i in range(16)]
        alltv = ["tv#%d" % i for i in range(16)]
        allti = ["ti#%d" % i for i in range(16)]
        allcv = ["cv#%d" % i for i in range(8)]

        def top16(src, srck, tmp, tmpk, vals, valk, idx=None, idxk=None):
            S.op("dve", lambda e: e.max(vals[:, 0:8], src), (srck,), (valk,))
            if idx is not None:
                S.op("dve", lambda e: e.max_index(idx[:, 0:8], vals[:, 0:8], src), (srck, valk), (idxk,))
            S.op("dve", lambda e: e.match_replace(tmp, vals[:, 0:8], src, NEG), (srck, valk), (tmpk,))
            S.op("dve", lambda e: e.max(vals[:, 8:16], tmp), (tmpk,), (valk,))
            if idx is not None:
                S.op("dve", lambda e: e.max_index(idx[:, 8:16], vals[:, 8:16], tmp), (tmpk, valk), (idxk,))

        for t in range(NOWN):
            gk_ = "gwa#%d" % t
            gw = gwa[:, t, :].rearrange("p (h k) -> p h k", k=16)
            for hp in range(16):
                mm(ps[:, hp // 4, (hp % 4) * 128:(hp % 4 + 1) * 128], qpT[:, hp, t * 128:(t + 1) * 128], keysT[:, hp, :],
                   True, True, ("mT", "keysT"), (PB[hp // 4],))
            act(scs[:].rearrange("p a b -> p (a b)"), psf(0, 4), AF.Copy, PB[0:4], allsc)
            for hp in range(16):
                top16(scs[:, hp, :], "scs#%d" % hp, scr[:, hp, :], "scr#%d" % hp, tv[:, hp, :], "tv#%d" % hp,
                      ti[:, hp, :], "ti#%d" % hp)
            cp("dve", tif[:], ti[:], allti, ("tif",))
            tv4 = tv[:].rearrange("p (h a) k -> p h a k", a=2)
            ti4 = tif[:].rearrange("p (h a) k -> p h a k", a=2)
            tt("dve", cand.rearrange("p h (a b) -> p h a b", b=16), tv4[:, :, 0, :].unsqueeze(3).to_broadcast([128, 8, 16, 16]),
               tv4[:, :, 1, :].unsqueeze(2).to_broadcast([128, 8, 16, 16]), ALU.add, alltv, allsc)
            ts("dve", tif[:].rearrange("p (h a) k -> p h a k", a=2)[:, :, 0, :], ti4[:, :, 0, :], 128.0, None, ALU.mult, None,
               ("tif",), ("tif",))
            for h in range(8):
                top16(cand[:, h, :], "scs#%d" % (2 * h), cand2[:, h, :], "scr#%d" % (2 * h), cv[:, h, :], "cv#%d" % h,
                      cpos[:, h, :], "cpos#%d" % h)
            allcp = ["cpos#%d" % i for i in range(8)]
            S.op("dve", lambda e: e.tensor_single_scalar(cab[:, 0], cpos[:], 4, ALU.logical_shift_right), allcp, ("cab",))
            S.op("dve", lambda e: e.tensor_single_scalar(cab[:, 1], cpos[:], 15, ALU.bitwise_and), allcp, ("cab",))
            cp("dve", cabf[:], cab[:], ("cab",), ("cabf",))
            for w_ in range(2):
                tt("dve", oh[:], iota[:, 0:16].unsqueeze(1).unsqueeze(1).to_broadcast([128, 8, 16, 16]),
                   cabf[:, w_].unsqueeze(3).to_broadcast([128, 8, 16, 16]), ALU.is_equal, ("iota", "cabf"), ("oh",))
                tt("dve", oh[:], oh[:], ti4[:, :, w_, :].unsqueeze(2).to_broadcast([128, 8, 16, 16]), ALU.mult,
                   ("oh", "tif"), ("oh",))
                red(esel[:, w_], oh[:], ALU.add, ("oh",), ("esel",))
            tt("dve", eidx[:].rearrange("p (h k) -> p h k", k=16), esel[:, 0], esel[:, 1], ALU.add, ("esel",), ("eidx",))
            ts("dve", eidx[:], eidx[:], 0.0, 16383.0, ALU.max, ALU.min, ("eidx",), ("eidx",))
            cp("dve", eida[:, t, :], eidx[:], ("eidx",), ("eida#%d" % t,))
            ts("dve", sm[:, 0:8], cv[:, :, 0], -1.0, None, ALU.mult, None, allcv, ("sm",))
            for h in range(8):
                act(gw[:, h, :], cv[:, h, :], AF.Exp, ("cv#%d" % h, "sm"), (gk_, "sm"), bias=sm[:, h:h + 1],
                    accum_out=sm[:, 8 + h:9 + h])
            guard("act", sm[:, 8:16], "sm")
            recip(sm[:, 16:24], sm[:, 8:16], ("sm",), ("sm",))
            tt("dve", gw, gw, sm[:, 16:24].unsqueeze(2).to_broadcast([128, 8, 16]), ALU.mult, (gk_, "sm"), (gk_,))
        for nm in ("scs", "scr", "tv", "ti", "tif", "cv", "cpos", "cab", "cabf", "oh", "esel", "iota", "sm", "eidx", "keysT",
                   "mT"):
            S.release(nm)

        av = S.alloc("av", [128, 128], F32)
        coef = S.alloc("coef", [128, 128], F32)
        junk2 = S.alloc("junk2", [128, D], BF)
        memset("pool", av[:], 0.0, ("av",))
        acc = h2f
        NG = 12
        ub = [S.alloc("ub%d" % i, [128, D], BF) for i in range(NG)]
        gi = 0
        for t in range(NOWN):
            xk_, nk, ek, gk_ = "x1#%d" % t, "n2s#%d" % t, "eida#%d" % t, "gwa#%d" % t
            stt(h2f[:], x1[:, t, :], n2s[:, 4 * t + 2:4 * t + 3], Prow, ALU.mult, ALU.mult, (xk_, nk, "rows#2"), ("h2f",))
            tt("pool", h2f[:], h2f[:], Qrow, ALU.add, ("h2f", "rows#1"), ("h2f",))
            for s_ in range(128):
                u_, uk = ub[gi % NG], "ub%d" % (gi % NG)
                gi += 1
                S.op("pool", lambda e, u_=u_, s_=s_, t=t: e.indirect_dma_start(
                    out=u_[:], out_offset=None, in_=u16[:, :],
                    in_offset=bass.IndirectOffsetOnAxis(ap=eida[:, t, s_:s_ + 1], axis=0)), [ek] + tabkeys[0:16], (uk,), dma=True)
                stt(junk2[:], u_[:], 1.0, h2f[:], ALU.mult, ALU.mult, (uk, "h2f"), ("junk2", "av"),
                    accum_out=av[:, s_:s_ + 1])
            guard("dve", av[:, 120:128], "av")
            act(coef[:], av[:], AF.Gelu, ("av",), ("coef",))
            tt("dve", coef[:], coef[:], gwa[:, t, :], ALU.mult, ("coef", gk_), ("coef",))
            for s_ in range(128):
                v_, vk_ = ub[gi % NG], "ub%d" % (gi % NG)
                gi += 1
                S.op("pool", lambda e, v_=v_, s_=s_, t=t: e.indirect_dma_start(
                    out=v_[:], out_offset=None, in_=v16[:, :],
                    in_offset=bass.IndirectOffsetOnAxis(ap=eida[:, t, s_:s_ + 1], axis=0)), [ek] + tabkeys[16:32], (vk_,), dma=True)
                if s_ == 0:
                    ts("dve", acc[:], v_[:], coef[:, 0:1], None, ALU.mult, None, (vk_, "coef"), ("h2f",))
                else:
                    stt(acc[:], v_[:], coef[:, s_:s_ + 1], acc[:], ALU.mult, ALU.add, (vk_, "coef", "h2f"), ("h2f",))
            tt("dve", acc[:], acc[:], g2row, ALU.mult, ("h2f", "rows#3"), ("h2f",))
            tt("pool", h2f[:], acc[:], x1[:, t, :], ALU.add, ("h2f", xk_), ("h2f",))
            dma("sp", out_d[t * 128:(t + 1) * 128, :], h2f[:], ("h2f",), ("out%d" % t,), is_out=True)

    S.emit()
    _LAST_SCHED[0] = S
    return nc


def _host_inputs(inp):
    f = lambda a: np.ascontiguousarray(np.asarray(a, dtype=np.float32))
    x = f(inp["x"]); ctx = f(inp["ctx"]); c = f(inp["c"]); c_ctx = f(inp["c_ctx"])
    t = np.arange(4096)
    row = (t // 64).astype(np.float32); col = (t % 64).astype(np.float32)
    inv = (np.float32(10000.0) ** (-np.arange(0, 32, 2, dtype=np.float32) / np.float32(32))).astype(np.float32)
    ar = (row[:, None] * inv).astype(np.float32); ac = (col[:, None] * inv).astype(np.float32)
    cr, sr, cc, sc = np.cos(ar), np.sin(ar), np.cos(ac), np.sin(ac)
    Cx = np.concatenate([cr, cr, cc, cc], axis=1).astype(np.float32)
    Sx = np.concatenate([-sr, sr, -sc, sc], axis=1).astype(np.float32)
    jj = np.arange(128)[:, None]; ii = np.arange(128)[None, :]
    A = (jj <= ii).astype(np.float32); B = (jj > ii).astype(np.float32)
    C_ = (jj >= ii).astype(np.float32); Dm = (jj < ii).astype(np.float32)
    sc16 = np.float32(-1.0 / 16.0)
    tri = np.stack([A * sc16, B * sc16, C_ * sc16, Dm * sc16, A, C_], axis=1).astype(np.float32)
    w2aug = np.zeros((33, 1024), np.float32)
    w2aug[0:16, 0:512] = f(inp["gla_w2_f"])[0]; w2aug[16:32, 512:] = f(inp["gla_w2_b"])[0]
    w2aug[32, 0:512] = f(inp["gla_b_f"])[0]; w2aug[32, 512:] = f(inp["gla_b_b"])[0]
    shared = {
        "ada_w": f(inp["ada_w"])[0], "adab_col": f(f(inp["ada_b"])[0].reshape(96, 128).T),
        "adab_row": f(f(inp["ada_b"])[0][None]), "n1g_col": f(f(inp["norm1_g"])[0].reshape(16, 128).T),
        "n2g_row": f(f(inp["norm2_g"])[0][None]), "w_in": f(inp["w_in"])[0],
        "qng_rep": f(np.broadcast_to(f(inp["da_qn_g"])[0], (128, 64))),
        "kng_rep": f(np.broadcast_to(f(inp["da_kn_g"])[0], (128, 64))),
        "lamp": f(np.broadcast_to(np.stack([f(inp["da_lam_q1"])[0], f(inp["da_lam_k1"])[0], f(inp["da_lam_q2"])[0],
                                            f(inp["da_lam_k2"])[0]]), (128, 4, 64))),
        "subln_col": f(f(inp["da_subln_g"])[0][:, None]), "w2aug": w2aug,
        "gnorm_rep": f(np.broadcast_to(f(inp["gla_norm_g"])[0], (128, 256))),
        "w_br_a": f(inp["w_br_a"])[0], "w_br_b": f(inp["w_br_b"])[0], "w_out": f(inp["w_out"])[0],
        "peer_wq": f(inp["peer_wq"])[0],
        "keys_l": f(f(inp["peer_keys"])[0].reshape(16, 128, 128).transpose(1, 0, 2)),
        "peer_u": f(inp["peer_u"])[0], "peer_v": f(inp["peer_v"])[0],
        "ident": np.eye(128, dtype=np.float32), "tri": tri,
        "iota256": f(np.broadcast_to(np.arange(256, dtype=np.float32), (128, 256))),
    }
    maps = []
    for core in range(8):
        b, s = core // 4, core % 4
        order = list(range(8 * s, 8 * s + 8)) + list(range(0, 8 * s)) + list(range(31, 8 * s + 7, -1))
        xb = x[b].reshape(32, 128, 2048)
        xp = np.concatenate([ctx[b].reshape(2, 128, 2048), xb[order]], axis=0).reshape(34 * 128, 2048)
        rc = np.concatenate([np.ones((256, 64), np.float32), Cx.reshape(32, 128, 64)[order].reshape(4096, 64)], axis=0)
        rs = np.concatenate([np.zeros((256, 64), np.float32), Sx.reshape(32, 128, 64)[order].reshape(4096, 64)], axis=0)
        msk = np.zeros((128, 27, 2), np.float32)
        msk[:, 0, 0] = 1; msk[:, 1, :] = 1; msk[:, 2, 1] = 1
        for j in range(24):
            msk[:, 3 + j, 0 if j < 8 * s else 1] = 1
        cT = np.stack([c[b], c_ctx]).reshape(2, 16, 128).transpose(2, 1, 0)
        m = dict(shared)
        m.update({"xp": f(xp), "ropeC": f(rc), "ropeS": f(rs), "msk": msk, "cT": f(cT)})
        maps.append(m)
    return maps


_NC_CACHE = {}


def kernel(**inputs):
    from concourse.bass_utils import run_bass_kernel_spmd
    maps = _host_inputs(inputs)
    if "nc" not in _NC_CACHE:
        _NC_CACHE["nc"] = build()
    nc = _NC_CACHE["nc"]
    res = run_bass_kernel_spmd(nc, maps, core_ids=list(range(8)))
    out = np.zeros((2, 4096, 2048), np.float32)
    for core in range(8):
        b, s = core // 4, core % 4
        out[b, 1024 * s:1024 * (s + 1)] = np.asarray(res.results[core]["out"]).reshape(1024, 2048)
    return out
```
